# Optimizing a Trainium2 kernel written in Bass

```python
import math
import jax, jax.numpy as jnp
from jax import lax
import numpy as np

D_MODEL = 1024
BATCH = 4
SEQ = 8192
DEPTH = 1

GRID_W = 64
CTX_LEN = 256
LN_EPS = 1e-6
DEEPNORM_ALPHA = (2 * DEPTH) ** 0.25
DEEPNORM_BETA = (8 * DEPTH) ** -0.25

GLA_HEADS = 4
GLA_DK = 64
GLA_DV = 128
GLA_KEY = GLA_HEADS * GLA_DK
GLA_VAL = GLA_HEADS * GLA_DV
GLA_RANK = 16
GLA_TAU = 16.0
GLA_CHUNK = 64

HY_WIDTH = D_MODEL - GLA_VAL
HY_ORDER = 2
HY_CONV = 3
HY_EMB = 33
HY_FH = 64
HY_TARGET = 1e-2
HY_FAST = 0.3
HY_SLOW = 1.5

MIX_WIDTH = GLA_VAL + HY_WIDTH
IN_SIZES = (GLA_KEY, GLA_KEY, GLA_VAL, GLA_VAL, GLA_RANK, GLA_RANK, (HY_ORDER + 1) * HY_WIDTH)
IN_WIDTH = sum(IN_SIZES)

N_EXPERTS = 32
TOP_K = 4
D_EXPERT = D_MODEL
SWIGLU_ALPHA = 1.702
SWIGLU_LIMIT = 7.0

kernel_name = 'hybrid_gla_hyena_moe_dit_block'


def _ln_stats(x):
    xf = x.astype(jnp.float32)
    mu = jnp.mean(xf, -1, keepdims=True)
    xc = xf - mu
    return xc * lax.rsqrt(jnp.mean(jnp.square(xc), -1, keepdims=True) + LN_EPS)


def modulate(x, shift, scale):
    return _ln_stats(x).astype(x.dtype) * (1 + scale) + shift


def post_norm(x, gain, bias):
    return (_ln_stats(x) * gain.astype(jnp.float32) + bias.astype(jnp.float32)).astype(x.dtype)


def mod_part(m, i):
    return m[..., i * D_MODEL:(i + 1) * D_MODEL]


def split_projection(u):
    return jnp.split(u, np.cumsum(IN_SIZES)[:-1].tolist(), axis=-1)


def to_heads(t, d):
    b, l, _ = t.shape
    return t.reshape(b, l, GLA_HEADS, d).transpose(0, 2, 1, 3)


def log_decay(a_low, wa, ba):
    z = a_low.astype(jnp.float32) @ wa.astype(jnp.float32) + ba.astype(jnp.float32)
    return to_heads(jax.nn.log_sigmoid(z) / GLA_TAU, GLA_DK)


def gla_chunked(q, k, v, log_a, s0):
    b, h, l, _ = q.shape
    n = l // GLA_CHUNK

    def chunks(t):
        return jnp.moveaxis(t.astype(jnp.float32).reshape(b, h, n, GLA_CHUNK, t.shape[-1]), 2, 0)

    lower = jnp.tril(jnp.ones((GLA_CHUNK, GLA_CHUNK), dtype=bool))[None, None, :, :, None]

    def step(s, inp):
        qc, kc, vc, gc = inp
        cum = jnp.cumsum(gc, axis=2)
        last = cum[:, :, -1:, :]
        o = jnp.einsum('bhid,bhdv->bhiv', qc * jnp.exp(cum), s)
        decay = jnp.exp(jnp.where(lower, cum[:, :, :, None, :] - cum[:, :, None, :, :], -jnp.inf))
        scores = jnp.einsum('bhid,bhjd,bhijd->bhij', qc, kc, decay)
        o = o + jnp.einsum('bhij,bhjv->bhiv', scores, vc)
        s = jnp.exp(last[:, :, 0, :])[..., None] * s + jnp.einsum('bhjd,bhjv->bhdv', kc * jnp.exp(last - cum), vc)
        return s, o

    s_fin, o = lax.scan(step, s0, (chunks(q), chunks(k), chunks(v), chunks(log_a)))
    return jnp.moveaxis(o, 0, 2).reshape(b, h, l, GLA_DV), s_fin


def gla_bidirectional(q, k, v, la_f, la_b, s0_f, s0_b):
    o_f, s_f = gla_chunked(q, k, v, la_f, s0_f)
    flip = lambda t: jnp.flip(t, axis=2)
    o_b, s_b = gla_chunked(flip(q), flip(k), flip(v), flip(la_b), s0_b)
    return o_f + flip(o_b), s_f, s_b


def gla_heads(q, k, v):
    return to_heads(q, GLA_DK) * (GLA_DK ** -0.5), to_heads(k, GLA_DK), to_heads(v, GLA_DV)


def gla_output(o, g, norm_g):
    o = o * lax.rsqrt(jnp.mean(jnp.square(o), -1, keepdims=True) + LN_EPS) * norm_g.astype(jnp.float32)
    b, _, l, _ = o.shape
    o = o.transpose(0, 2, 1, 3).reshape(b, l, GLA_VAL).astype(g.dtype)
    return o * jax.nn.silu(g)


def short_conv_rows(u, w, bias, rows):
    b, l, ch = u.shape
    row_len = l // rows
    up = jnp.pad(u.reshape(b, rows, row_len, ch), ((0, 0), (0, 0), (1, 1), (0, 0)))
    y = sum(up[:, :, i:i + row_len] * w[i] for i in range(HY_CONV)) + bias
    return y.reshape(b, l, ch)


def hyena_filters(length, w1, b1, w2, b2, w_out, freq):
    f32 = jnp.float32
    t = jnp.arange(length, dtype=f32)
    t_norm = t / (length - 1)
    bands = (HY_EMB - 1) // 2
    f = jnp.linspace(1e-4, bands - 1, bands, dtype=f32)
    ang = (2.0 * math.pi * t / length)[:, None] * f[None, :]
    z = jnp.concatenate([t_norm[:, None], jnp.cos(ang), -jnp.sin(ang)], -1)
    fr = freq.astype(f32)
    hid = jnp.sin(fr * (z @ w1.astype(f32) + b1.astype(f32)))
    hid = jnp.sin(fr * (hid @ w2.astype(f32) + b2.astype(f32)))
    h = (hid @ w_out.astype(f32)).reshape(length, HY_ORDER, 2, HY_WIDTH)
    deltas = jnp.linspace(math.log(HY_TARGET) / HY_SLOW, math.log(HY_TARGET) / HY_FAST, HY_WIDTH, dtype=f32)
    window = jnp.exp(-t_norm[:, None] * jnp.abs(deltas)[None, :])
    return h * window[:, None, None, :]


def long_conv_bidir(u, h_fwd, h_bwd, d_bias):
    l = u.shape[1]
    two_sided = jnp.concatenate([h_fwd, jnp.zeros_like(h_fwd[:1]), jnp.flip(h_bwd[1:], 0)], 0)
    uf = u.astype(jnp.float32)
    spec = jnp.fft.rfft(uf, n=2 * l, axis=1) * jnp.fft.rfft(two_sided, axis=0)[None]
    y = jnp.fft.irfft(spec, n=2 * l, axis=1)[:, :l]
    return (y + uf * d_bias.astype(jnp.float32)).astype(u.dtype)


def hyena(u, filters, conv_w, conv_b, d_bias, rows):
    u = short_conv_rows(u, conv_w, conv_b, rows)
    v, x1, x2 = jnp.split(u, HY_ORDER + 1, axis=-1)
    z = x1 * long_conv_bidir(v, filters[:, 0, 0], filters[:, 0, 1], d_bias[0])
    return x2 * long_conv_bidir(z, filters[:, 1, 0], filters[:, 1, 1], d_bias[1])


def clamped_swiglu(hid):
    glu, lin = hid[..., ::2], hid[..., 1::2]
    glu = jnp.minimum(glu, SWIGLU_LIMIT)
    lin = jnp.clip(lin, -SWIGLU_LIMIT, SWIGLU_LIMIT)
    return glu * jax.nn.sigmoid(SWIGLU_ALPHA * glu) * (lin + 1)


def moe(h, router_w, router_b, w1, b1, w2, b2):
    b, l, d = h.shape
    t = h.reshape(b * l, d)
    logits = (t @ router_w + router_b).astype(jnp.float32)
    top_v, top_i = lax.top_k(logits, TOP_K)
    top_w = jax.nn.softmax(top_v, axis=-1)
    gates = jnp.sum(jax.nn.one_hot(top_i, N_EXPERTS, dtype=jnp.float32) * top_w[..., None], axis=1)
    out = jnp.zeros((b * l, d), jnp.float32)
    for e in range(N_EXPERTS):
        y_e = clamped_swiglu(t @ w1[e] + b1[e]) @ w2[e] + b2[e]
        out = out + gates[:, e:e + 1] * y_e.astype(jnp.float32)
    return out.astype(h.dtype).reshape(b, l, d)


def setup_inputs(seed: int = 0) -> dict:
    key = jax.random.key(seed)
    ks = iter(jax.random.split(key, 40))
    nrm = lambda shape, std: std * jax.random.normal(next(ks), shape, jnp.float32)
    D = D_MODEL
    return {
        'x': nrm((BATCH, SEQ, D), 1.0),
        'c': nrm((BATCH, D), 1.0),
        'ctx': nrm((BATCH, CTX_LEN, D), 1.0),
        'c_ctx': nrm((D,), 1.0),
        'ada_w': nrm((DEPTH, D, 6 * D), 0.5 * D ** -0.5),
        'ada_b': nrm((DEPTH, 6 * D), 0.01),
        'w_in': nrm((DEPTH, D, IN_WIDTH), D ** -0.5),
        'gla_wa_f': nrm((DEPTH, GLA_RANK, GLA_KEY), GLA_RANK ** -0.5),
        'gla_ba_f': nrm((DEPTH, GLA_KEY), 0.1),
        'gla_wa_b': nrm((DEPTH, GLA_RANK, GLA_KEY), GLA_RANK ** -0.5),
        'gla_ba_b': nrm((DEPTH, GLA_KEY), 0.1),
        'gla_norm_g': 1.0 + nrm((DEPTH, GLA_DV), 0.01),
        'hy_conv_w': nrm((DEPTH, HY_CONV, (HY_ORDER + 1) * HY_WIDTH), HY_CONV ** -0.5),
        'hy_conv_b': nrm((DEPTH, (HY_ORDER + 1) * HY_WIDTH), 0.01),
        'hy_flt_w1': nrm((DEPTH, HY_EMB, HY_FH), HY_EMB ** -0.5),
        'hy_flt_b1': nrm((DEPTH, HY_FH), 0.1),
        'hy_flt_w2': nrm((DEPTH, HY_FH, HY_FH), HY_FH ** -0.5),
        'hy_flt_b2': nrm((DEPTH, HY_FH), 0.1),
        'hy_flt_wout': nrm((DEPTH, HY_FH, HY_ORDER * 2 * HY_WIDTH), 0.05 * HY_FH ** -0.5),
        'hy_flt_freq': 1.0 + nrm((DEPTH, HY_FH), 0.01),
        'hy_bias_d': nrm((DEPTH, HY_ORDER, HY_WIDTH), 1.0),
        'w_out': nrm((DEPTH, MIX_WIDTH, D), DEEPNORM_BETA * MIX_WIDTH ** -0.5),
        'ln1_g': 1.0 + nrm((DEPTH, D), 0.01),
        'ln1_b': nrm((DEPTH, D), 0.01),
        'router_w': nrm((DEPTH, D, N_EXPERTS), D ** -0.5),
        'router_b': nrm((DEPTH, N_EXPERTS), 0.01),
        'exp_w1': nrm((DEPTH, N_EXPERTS, D, 2 * D_EXPERT), D ** -0.5),
        'exp_b1': nrm((DEPTH, N_EXPERTS, 2 * D_EXPERT), 0.01),
        'exp_w2': nrm((DEPTH, N_EXPERTS, D_EXPERT, D), DEEPNORM_BETA * D_EXPERT ** -0.5),
        'exp_b2': nrm((DEPTH, N_EXPERTS, D), 0.01),
        'ln2_g': 1.0 + nrm((DEPTH, D), 0.01),
        'ln2_b': nrm((DEPTH, D), 0.01),
    }


def reference(x, c, ctx, c_ctx, ada_w, ada_b, w_in, gla_wa_f, gla_ba_f, gla_wa_b, gla_ba_b, gla_norm_g,
              hy_conv_w, hy_conv_b, hy_flt_w1, hy_flt_b1, hy_flt_w2, hy_flt_b2, hy_flt_wout, hy_flt_freq,
              hy_bias_d, w_out, ln1_g, ln1_b, router_w, router_b, exp_w1, exp_b1, exp_w2, exp_b2, ln2_g, ln2_b):
    batch, seq_len, _ = x.shape
    ctx_len = ctx.shape[1]
    rows = seq_len // GRID_W
    for l in range(DEPTH):
        update_ctx = l < DEPTH - 1
        mod_x = (jax.nn.silu(c) @ ada_w[l] + ada_b[l])[:, None, :]
        mod_c = (jax.nn.silu(c_ctx) @ ada_w[l] + ada_b[l])[None, None, :]
        flt = functools_free = (hy_flt_w1[l], hy_flt_b1[l], hy_flt_w2[l], hy_flt_b2[l], hy_flt_wout[l], hy_flt_freq[l])

        h = modulate(x, mod_part(mod_x, 0), mod_part(mod_x, 1))
        hc = modulate(ctx, mod_part(mod_c, 0), mod_part(mod_c, 1))
        q, k, v, g, a_f, a_b, hy_u = split_projection(h @ w_in[l])
        qc, kc, vc, gc, a_fc, a_bc, hy_uc = split_projection(hc @ w_in[l])

        zero = jnp.zeros((batch, GLA_HEADS, GLA_DK, GLA_DV), jnp.float32)
        oc, s_f, s_b = gla_bidirectional(*gla_heads(qc, kc, vc), log_decay(a_fc, gla_wa_f[l], gla_ba_f[l]),
                                         log_decay(a_bc, gla_wa_b[l], gla_ba_b[l]), zero, zero)
        o, _, _ = gla_bidirectional(*gla_heads(q, k, v), log_decay(a_f, gla_wa_f[l], gla_ba_f[l]),
                                    log_decay(a_b, gla_wa_b[l], gla_ba_b[l]), s_f, s_b)
        y_gla = gla_output(o, g, gla_norm_g[l])

        y_hy = hyena(hy_u, hyena_filters(seq_len, *flt), hy_conv_w[l], hy_conv_b[l], hy_bias_d[l], rows)
        f = jnp.concatenate([y_gla, y_hy], axis=-1) @ w_out[l]
        x_mid = post_norm(DEEPNORM_ALPHA * x + mod_part(mod_x, 2) * f, ln1_g[l], ln1_b[l])

        h2 = modulate(x_mid, mod_part(mod_x, 3), mod_part(mod_x, 4))
        y_moe = moe(h2, router_w[l], router_b[l], exp_w1[l], exp_b1[l], exp_w2[l], exp_b2[l])
        x_new = post_norm(DEEPNORM_ALPHA * x_mid + mod_part(mod_x, 5) * y_moe, ln2_g[l], ln2_b[l])

        if update_ctx:
            yc_hy = hyena(hy_uc, hyena_filters(ctx_len, *flt), hy_conv_w[l], hy_conv_b[l], hy_bias_d[l], 1)
            fc = jnp.concatenate([gla_output(oc, gc, gla_norm_g[l]), yc_hy], axis=-1) @ w_out[l]
            ctx_mid = post_norm(DEEPNORM_ALPHA * ctx + mod_part(mod_c, 2) * fc, ln1_g[l], ln1_b[l])
            hc2 = modulate(ctx_mid, mod_part(mod_c, 3), mod_part(mod_c, 4))
            yc_moe = moe(hc2, router_w[l], router_b[l], exp_w1[l], exp_b1[l], exp_w2[l], exp_b2[l])
            ctx = post_norm(DEEPNORM_ALPHA * ctx_mid + mod_part(mod_c, 5) * yc_moe, ln2_g[l], ln2_b[l])
        x = x_new
    return x
```

```python
import numpy as np
import concourse.bass as bass
import concourse.mybir as mybir
from concourse.bass_utils import run_bass_kernel_spmd
from contextlib import ExitStack

F32 = mybir.dt.float32
BF16 = mybir.dt.bfloat16
AF = mybir.ActivationFunctionType
OP = mybir.AluOpType

D = 1024
SEQ = 8192
OWN = 4096
NE = 32
ALPHA = 2.0 ** 0.25
EPS = 1e-6
DEBUG = {}


class Buf:
    __slots__ = ("name", "t", "w", "r", "dsem", "dcnt", "dkey")

    def __init__(self, name, t=None):
        self.name = name
        self.t = t
        self.w = None
        self.r = {}
        self.dsem = None
        self.dcnt = 0
        self.dkey = None

    def __getitem__(self, idx):
        return self.t[idx]


class PB:
    __slots__ = ("S", "idx", "gen")

    def __init__(self, S, idx, gen):
        self.S = S
        self.idx = idx
        self.gen = gen

    def __getitem__(self, i):
        return self.S.banks[self.idx].t[i]


class Sched:
    def __init__(self, nc, stack):
        self.nc = nc
        self.stack = stack
        self.engs = {}
        for nm in ("pe", "act", "dve", "pool", "sp"):
            sem = stack.enter_context(nc.semaphore(f"s_{nm}"))
            self.engs[nm] = dict(key=f"E{nm}", sem=sem, cnt=0, ops=[], seen={})
        self.nd = 0
        self.dsems = []
        self.freesems = []
        self.swsems = set()
        self.bank_i = 0
        self.banks = []
        self.bankgen = {}

    def sb(self, name, shape, dt, stack=None):
        self.nsb = getattr(self, "nsb", 0) + 1
        t = (stack or self.stack).enter_context(self.nc.sbuf_tensor(f"{name}_s{self.nsb}", list(shape), dt))
        b = Buf(name, t)
        if stack is not None:
            stack.callback(self._release, b)
        return b

    def _release(self, b):
        if b.dsem is not None:
            if b.dkey not in self.swsems:
                self.freesems.append((b.dsem, b.dkey, b.dcnt))
            b.dsem = None

    def ps(self, name, shape, dt):
        t = self.stack.enter_context(self.nc.psum_tensor(name, list(shape), dt))
        return Buf(name, t)

    def dram(self, name, shape, dt, kind="Internal"):
        t = self.nc.dram_tensor(name, list(shape), dt, kind=kind)
        return Buf(name, t.ap())

    def bank(self):
        i = self.bank_i % len(self.banks)
        self.bank_i += 1
        self.bankgen[i] = self.bankgen.get(i, 0) + 1
        return PB(self, i, self.bankgen[i])

    def _norm(self, bs):
        out = []
        for x in bs:
            if isinstance(x, PB):
                assert x.gen == self.bankgen[x.idx], f"stale PSUM bank ref {x.idx}"
                out.append(self.banks[x.idx])
            else:
                out.append(x)
        return out

    def _dsem(self, b, q="sp"):
        if b.dsem is None and self.freesems and q != "pool":
            b.dsem, b.dkey, b.dcnt = self.freesems.pop()
            self.dsems.append(b)
        if b.dsem is None:
            self.nd += 1
            b.dsem = self.stack.enter_context(self.nc.semaphore(f"d{self.nd}"))
            b.dkey = f"D{self.nd}"
            self.dsems.append(b)
        return b.dsem

    def _waits(self, E, reads, writes, skipkey=None):
        waits = {}
        is_pe = E["key"] == "Epe"

        def need(rec, waw=False):
            if rec is None:
                return
            key, h, val = rec
            if key == skipkey:
                return
            if key == E["key"] and is_pe and waw:
                return
            if E["seen"].get(key, 0) >= val:
                return
            if key in waits and waits[key][1] >= val:
                return
            waits[key] = (h, val)

        for b in reads:
            need(b.w)
            if b.name.startswith("bank"):
                for rec in b.r.values():
                    if rec[0] != E["key"]:
                        need(rec)
        for b in writes:
            need(b.w, waw=True)
            for rec in b.r.values():
                need(rec)
        for key, (h, val) in waits.items():
            E["seen"][key] = val
        return list(waits.values())

    def op(self, eng, fn, reads=(), writes=()):
        E = self.engs[eng]
        reads = self._norm(reads)
        writes = self._norm(writes)
        waits = self._waits(E, reads, writes)
        E["cnt"] += 1
        rec = (E["key"], E["sem"], E["cnt"])
        E["ops"].append((waits, fn, E["sem"], 1))
        for b in reads:
            b.r[E["key"]] = rec
        for b in writes:
            b.w = rec
            b.r = {}

    def dma(self, q, out_ap, in_ap, reads=(), writes=(), owner=None, **kw):
        Q = self.engs[q]
        reads = self._norm(reads)
        writes = self._norm(writes)
        if owner is None:
            cands = [b for b in list(writes) + list(reads) if not b.name.endswith("_d")]
            owner = cands[0]
        dsem = self._dsem(owner, q)
        if q == "pool":
            self.swsems.add(owner.dkey)
        waits = self._waits(Q, reads, writes, skipkey=owner.dkey)
        owner.dcnt += 16
        rec = (owner.dkey, dsem, owner.dcnt)

        def fn(e, out_ap=out_ap, in_ap=in_ap, kw=kw):
            return e.dma_start(out=out_ap, in_=in_ap, **kw)

        Q["ops"].append((waits, fn, dsem, 16))
        for b in reads:
            b.r[owner.dkey] = rec
        for b in writes:
            b.w = rec
            b.r = {}

    def barrier(self):
        targets = []
        for nm, E in self.engs.items():
            if E["cnt"] > 0:
                targets.append((E["key"], E["sem"], E["cnt"]))
        for b in self.dsems:
            if b.dcnt > 0 and b.dsem is not None:
                targets.append((b.dkey, b.dsem, b.dcnt))
        self.dsems = [b for b in self.dsems if b.dsem is not None]
        for nm, E in self.engs.items():
            waits = []
            for key, h, val in targets:
                if key == E["key"]:
                    continue
                if E["seen"].get(key, 0) >= val:
                    continue
                E["seen"][key] = val
                waits.append((h, val))
            if waits:
                E["ops"].append((waits, None, None, 0))

    def emit(self):
        nc = self.nc
        self.barrier()
        with nc.Block() as block:
            def runner(nm):
                E = self.engs[nm]

                def body(e):
                    for waits, fn, sem, inc in E["ops"]:
                        for h, val in waits:
                            e.wait_ge(h, val)
                        if fn is not None:
                            inst = fn(e)
                            inst.then_inc(sem, inc)
                return body

            block.tensor(runner("pe"))
            block.scalar(runner("act"))
            block.vector(runner("dve"))
            block.gpsimd(runner("pool"))
            block.sync(runner("sp"))

    def mm(self, ob, out, lb, lhsT, rb, rhs, start, stop):
        self.op("pe", lambda e: e.matmul(out, lhsT=lhsT, rhs=rhs, start=start, stop=stop), reads=[lb, rb], writes=[ob])

    def tr(self, ob, out, ib, in_, idb, ident):
        self.op("pe", lambda e: e.transpose(out, in_, ident), reads=[ib, idb], writes=[ob])

    def act(self, ob, out, ib, in_, func, bias=None, scale=None, extra=(), accum=None, accb=None):
        kw = {}
        if bias is not None:
            kw["bias"] = bias
        if scale is not None:
            kw["scale"] = scale
        if accum is not None:
            kw["accum_out"] = accum
        w = [ob] + ([accb] if accb is not None else [])
        self.op("act", lambda e: e.activation(out=out, in_=in_, func=func, **kw), reads=[ib] + list(extra), writes=w)

    def ts(self, eng, ob, out, ib, in0, s1, s2, op0, op1=None, extra=()):
        if op1 is None:
            self.op(eng, lambda e: e.tensor_scalar(out=out, in0=in0, scalar1=s1, scalar2=None, op0=op0), reads=[ib] + list(extra), writes=[ob])
        else:
            self.op(eng, lambda e: e.tensor_scalar(out=out, in0=in0, scalar1=s1, scalar2=s2, op0=op0, op1=op1), reads=[ib] + list(extra), writes=[ob])

    def tt(self, eng, ob, out, ab, in0, bb, in1, op):
        self.op(eng, lambda e: e.tensor_tensor(out=out, in0=in0, in1=in1, op=op), reads=[ab, bb], writes=[ob])

    def stt(self, ob, out, ab, in0, scalar, bb, in1, op0, op1, extra=()):
        self.op("dve", lambda e: e.scalar_tensor_tensor(out=out, in0=in0, scalar=scalar, in1=in1, op0=op0, op1=op1),
                reads=[ab, bb] + list(extra), writes=[ob])

    def cp(self, eng, ob, out, ib, in_):
        if eng == "act":
            self.op("act", lambda e: e.activation(out=out, in_=in_, func=AF.Copy), reads=[ib], writes=[ob])
            return
        self.op(eng, lambda e: e.tensor_copy(out=out, in_=in_), reads=[ib], writes=[ob])

    def memset(self, eng, ob, ap, val):
        self.op(eng, lambda e: e.memset(ap, val), writes=[ob])


def ln_stats(S, xb, xap, stats, mv, rstd, epsb):
    for i in range(2):
        S.op("dve", (lambda i: lambda e: e.bn_stats(out=stats[:, i * 6:(i + 1) * 6], in_=xap[:, i * 512:(i + 1) * 512]))(i),
             reads=[xb], writes=[stats])
    S.op("dve", lambda e: e.bn_aggr(out=mv[:, 0:2], in_=stats[:, 0:12]), reads=[stats], writes=[mv])
    S.act(rstd, rstd[:, 0:1], mv, mv[:, 1:2], AF.Sqrt, bias=epsb[:, 0:1], scale=1.0, extra=[epsb])
    S.op("dve", lambda e: e.reciprocal(out=rstd[:, 0:1], in_=rstd[:, 0:1]), reads=[rstd], writes=[rstd])


def build_mod(S, st, io, C):
    nc = S.nc
    modT = S.sb("modT", [128, 6, 8], F32)
    modTc = S.sb("modTc", [128, 6, 8], F32)
    g1bc = S.sb("g1bc", [128, 1024], F32)
    g2bc = S.sb("g2bc", [128, 1024], F32)
    with ExitStack() as ls:
        cc = S.sb("cc", [128, 8, 2], F32, ls)
        sc = S.sb("sc", [128, 8, 2], F32, ls)
        scb = S.sb("scb", [128, 8, 128], F32, ls)
        adab = S.sb("adab", [128, 6, 8], F32, ls)
        adabrow = S.sb("adabrow", [1, 6144], F32, ls)
        slabs = [S.sb(f"slab{i}", [128, 8, 512], F32, ls) for i in range(2)]
        S.dma("sp", cc[:, :, :], io["c2"], writes=[cc])
        S.dma("sp", adab[:, :, :], io["ada_bT"], writes=[adab])
        S.dma("sp", adabrow[:, :], io["ada_b"], writes=[adabrow])
        S.act(sc, sc[:, :, :], cc, cc[:, :, :], AF.Silu)
        for kc in range(8):
            S.ts("dve", scb, scb[:, kc, :], C["ones"], C["ones"][:, 0:128], sc[:, kc, 0:1], None, OP.mult, extra=[sc])
        for s in range(12):
            part, half = s // 2, s % 2
            slab = slabs[s % 2]
            for kh in range(2):
                S.dma("sp", slab[:, kh * 4:(kh + 1) * 4, :],
                      io["ada_w"][kh * 512:(kh + 1) * 512, s * 512:(s + 1) * 512].rearrange("(kc k) n -> k kc n", k=128),
                      writes=[slab])
            pb = S.bank()
            for dc in range(4):
                for kc in range(8):
                    S.mm(pb, pb[:, dc * 2:dc * 2 + 2], slab, slab[:, kc, dc * 128:(dc + 1) * 128], sc, sc[:, kc, 0:2], kc == 0, kc == 7)
            for dc in range(4):
                dci = half * 4 + dc
                S.ts("dve", modT, modT[:, part, dci:dci + 1], pb, pb[:, dc * 2:dc * 2 + 1], adab[:, part, dci:dci + 1], None, OP.add, extra=[adab])
                S.ts("dve", modTc, modTc[:, part, dci:dci + 1], pb, pb[:, dc * 2 + 1:dc * 2 + 2], adab[:, part, dci:dci + 1], None, OP.add, extra=[adab])
            if part in (2, 5):
                pb2 = S.bank()
                for kc in range(8):
                    S.mm(pb2, pb2[:, :], scb, scb[:, kc, :], slab, slab[:, kc, :], kc == 0, False)
                S.mm(pb2, pb2[:, :], C["ones"], C["ones"][0:1, 0:128], adabrow, adabrow[0:1, s * 512:(s + 1) * 512], False, True)
                dst = g1bc if part == 2 else g2bc
                S.cp("dve", dst, dst[:, half * 512:(half + 1) * 512], pb2, pb2[:, :])
        S.barrier()
    return dict(modT=modT, modTc=modTc, g1bc=g1bc, g2bc=g2bc)


def bcast_row(S, C, dst, row_ap, rowbuf, n):
    S.dma("sp", rowbuf[0:1, 0:n], row_ap, writes=[rowbuf])
    for h in range(0, n, 512):
        w = min(512, n - h)
        pb = S.bank()
        S.mm(pb, pb[:, 0:w], C["ones"], C["ones"][0:1, 0:128], rowbuf, rowbuf[0:1, h:h + w], True, True)
        S.cp("dve", dst, dst[:, h:h + w], pb, pb[:, 0:w])


def phase_B(S, io, C, M, gates, NT=32):
    nc = S.nc
    modT, g1bc, g2bc = M["modT"], M["g1bc"], M["g2bc"]
    xmid_d = S.dram("xmid_d", [NT, 128, 1024], F32)
    h2T_d = S.dram("h2T_d", [NT, 128, 8, 128], BF16)
    with ExitStack() as ls:
        wo = S.sb("wo", [128, 8, 1024], BF16, ls)
        rw = S.sb("rw", [128, 8, 32], F32, ls)
        rbrow = S.sb("rbrow", [1, 32], F32, ls)
        rowbuf = S.sb("rowbuf", [1, 1024], F32, ls)
        ln1g = S.sb("ln1g", [128, 1024], F32, ls)
        ln1b = S.sb("ln1b", [128, 1024], F32, ls)
        sc4 = S.sb("sc4", [128, 8], F32, ls)
        xt = [S.sb(f"xt{i}", [128, 1024], F32, ls) for i in range(2)]
        ym = [S.sb(f"ym{i}", [128, 8, 128], BF16, ls) for i in range(2)]
        r = [S.sb(f"r{i}", [128, 1024], F32, ls) for i in range(2)]
        xm = [S.sb(f"xm{i}", [128, 1024], F32, ls) for i in range(2)]
        xn2 = [S.sb(f"xn2{i}", [128, 1024], F32, ls) for i in range(2)]
        h2b = [S.sb(f"h2b{i}", [128, 8, 128], BF16, ls) for i in range(2)]
        h2f = [S.sb(f"h2f{i}", [128, 8, 128], F32, ls) for i in range(2)]
        def ring2(nm, shp):
            return [S.sb(f"{nm}{i}", shp, F32, ls) for i in range(2)]
        statsA, mvA, rstdA = ring2("statsA", [128, 12]), ring2("mvA", [128, 2]), ring2("rstdA", [128, 1])
        statsB, mvB, rstdB = ring2("statsB", [128, 12]), ring2("mvB", [128, 2]), ring2("rstdB", [128, 1])
        lg_, t8_, nv1_, msk_, ex_, ssum_ = (ring2("lg", [128, 32]), ring2("t8", [128, 8]), ring2("nv1", [128, 1]),
                                            ring2("msk", [128, 32]), ring2("ex", [128, 32]), ring2("ssum", [128, 1]))
        S.dma("pool", wo[:, :, :], io["w_out"].rearrange("(kc k) n -> k kc n", k=128), writes=[wo])
        S.dma("sp", rw[:, :, :], io["router_w"].rearrange("(kc k) n -> k kc n", k=128), writes=[rw])
        S.dma("sp", rbrow[:, :], io["router_b"], writes=[rbrow])
        bcast_row(S, C, ln1g, io["ln1_g"], rowbuf, 1024)
        bcast_row(S, C, ln1b, io["ln1_b"], rowbuf, 1024)
        S.ts("dve", sc4, sc4[:, :], modT, modT[:, 4, :], 1.0, None, OP.add)
        for tt in range(NT):
            i = tt % 2
            S.dma("sp", xt[i][:, :], io["x_own"][tt * 128:(tt + 1) * 128, :], writes=[xt[i]])
            S.dma("sp", ym[i][:, :, :], io["ymT"][tt], writes=[ym[i]], reads=[io["ymT_buf"]])
            pf = [S.bank(), S.bank()]
            for h in range(2):
                for kc in range(8):
                    S.mm(pf[h], pf[h][:, :], ym[i], ym[i][:, kc, :], wo, wo[:, kc, h * 512:(h + 1) * 512], kc == 0, kc == 7)
            for h in range(2):
                S.tt("dve", r[i], r[i][:, h * 512:(h + 1) * 512], pf[h], pf[h][:, :], g1bc, g1bc[:, h * 512:(h + 1) * 512], OP.mult)
            S.stt(r[i], r[i][:, :], xt[i], xt[i][:, :], ALPHA, r[i], r[i][:, :], OP.mult, OP.add)
            stats, mv, rstd = statsA[i], mvA[i], rstdA[i]
            ln_stats(S, r[i], r[i], stats, mv, rstd, C["eps"])
            S.ts("dve", xm[i], xm[i][:, :], r[i], r[i][:, :], mv[:, 0:1], rstd[:, 0:1], OP.subtract, OP.mult, extra=[mv, rstd])
            S.tt("pool", xm[i], xm[i][:, :], xm[i], xm[i][:, :], ln1g, ln1g[:, :], OP.mult)
            S.tt("pool", xm[i], xm[i][:, :], xm[i], xm[i][:, :], ln1b, ln1b[:, :], OP.add)
            S.dma("sp", xmid_d[tt], xm[i][:, :], reads=[xm[i]], writes=[xmid_d])
            stats, mv, rstd = statsB[i], mvB[i], rstdB[i]
            ln_stats(S, xm[i], xm[i], stats, mv, rstd, C["eps"])
            S.ts("dve", xn2[i], xn2[i][:, :], xm[i], xm[i][:, :], mv[:, 0:1], rstd[:, 0:1], OP.subtract, OP.mult, extra=[mv, rstd])
            pt = [S.bank(), S.bank()]
            for kc in range(8):
                p = pt[kc // 4]
                S.tr(p, p[:, (kc % 4) * 128:(kc % 4 + 1) * 128], xn2[i], xn2[i][:, kc * 128:(kc + 1) * 128], C["ident"], C["ident"][:, :])
            for kc in range(8):
                p = pt[kc // 4]
                src_ = p[:, (kc % 4) * 128:(kc % 4 + 1) * 128]
                S.ts("dve", h2f[i], h2f[i][:, kc, :], p, src_, sc4[:, kc:kc + 1], modT[:, 3, kc:kc + 1], OP.mult, OP.add, extra=[modT, sc4])
            S.cp("act", h2b[i], h2b[i][:, :, :], h2f[i], h2f[i][:, :, :])
            S.dma("sp", h2T_d[tt], h2b[i][:, :, :], reads=[h2b[i]], writes=[h2T_d])
            pl = S.bank()
            for kc in range(8):
                S.mm(pl, pl[:, 0:32], h2f[i], h2f[i][:, kc, :], rw, rw[:, kc, :], kc == 0, False)
            S.mm(pl, pl[:, 0:32], C["ones"], C["ones"][0:1, 0:128], rbrow, rbrow[0:1, :], False, True)
            lg, t8, nv1, msk, ex, ssum = lg_[i], t8_[i], nv1_[i], msk_[i], ex_[i], ssum_[i]
            S.cp("dve", lg, lg[:, :], pl, pl[:, 0:32])
            S.op("dve", (lambda a_, b_: lambda e: e.max(out=a_[:, :], in_=b_[:, :]))(t8, lg), reads=[lg], writes=[t8])
            S.ts("dve", nv1, nv1[:, :], t8, t8[:, 0:1], -1.0, None, OP.mult)
            S.ts("dve", msk, msk[:, :], lg, lg[:, :], t8[:, 3:4], None, OP.is_ge, extra=[t8])
            S.act(ex, ex[:, :], lg, lg[:, :], AF.Exp, bias=nv1[:, 0:1], scale=1.0, extra=[nv1])
            S.tt("dve", ex, ex[:, :], ex, ex[:, :], msk, msk[:, :], OP.mult)
            S.op("dve", (lambda a_, b_: lambda e: e.reduce_sum(out=a_[:, :], in_=b_[:, :], axis=mybir.AxisListType.X))(ssum, ex), reads=[ex], writes=[ssum])
            S.op("dve", (lambda a_: lambda e: e.reciprocal(out=a_[:, :], in_=a_[:, :]))(ssum), reads=[ssum], writes=[ssum])
            S.ts("dve", gates, gates[:, tt, :], ex, ex[:, :], ssum[:, 0:1], None, OP.mult, extra=[ssum])
        S.barrier()
    if DEBUG.get("skip_moe"):
        return
    with ExitStack() as ls:
        QT = 8
        acc = S.sb("acc", [128, QT, 1024], F32, ls)
        h2T = S.sb("h2T", [128, 8, QT * 128], BF16, ls)
        w1 = [S.sb(f"w1_{i}", [128, 8, 2048], BF16, ls) for i in range(2)]
        w2 = [S.sb(f"w2_{i}", [128, 8, 1024], BF16, ls) for i in range(2)]
        actT = [S.sb(f"actT{i}", [128, 8, 512], BF16, ls) for i in range(2)]
        gsb = [S.sb(f"g{i}", [128, 512], F32, ls) for i in range(2)]
        gss = [S.sb(f"gs{i}", [128, 512], F32, ls) for i in range(2)]
        asb = [S.sb(f"a{i}", [128, 512], F32, ls) for i in range(2)]
        b1 = S.sb("b1", [128, NE, 8, 2], F32, ls)
        b2s = S.sb("b2s", [32, 1024], F32, ls)
        gT = S.sb("gT", [32, 128], F32, ls)
        ln2g = S.sb("ln2g", [128, 1024], F32, ls)
        ln2b = S.sb("ln2b", [128, 1024], F32, ls)
        stats = S.sb("stats2", [128, 12], F32, ls)
        mv = S.sb("mv2", [128, 2], F32, ls)
        rstd = S.sb("rstd2", [128, 1], F32, ls)
        with ExitStack() as l2:
            rowbuf = S.sb("rowbuf2", [1, 1024], F32, l2)
            bcast_row(S, C, ln2g, io["ln2_g"], rowbuf, 1024)
            bcast_row(S, C, ln2b, io["ln2_b"], rowbuf, 1024)
            S.barrier()
        S.dma("sp", b1[:, :, :, :], io["b1T"], writes=[b1])
        S.ts("dve", b1, b1[:, :, :, 1:2], b1, b1[:, :, :, 1:2], 1.0, None, OP.add)
        S.dma("sp", b2s[:, :], io["exp_b2"], writes=[b2s])
        S.tt("dve", b2s, b2s[:, :], b2s, b2s[:, :], g2bc, g2bc[0:32, :], OP.mult)
        ei = 0
        for qt in range(NT // QT):
            for t in range(QT):
                tt = qt * QT + t
                S.dma("sp", h2T[:, :, t * 128:(t + 1) * 128], h2T_d[tt], writes=[h2T], reads=[h2T_d])
                S.dma("sp", acc[:, t, :], xmid_d[tt], writes=[acc], reads=[xmid_d])
            S.ts("dve", acc, acc[:, :, :], acc, acc[:, :, :], ALPHA, None, OP.mult)
            for e in range(NE):
                wb1 = w1[ei % 2]
                wb2 = w2[ei % 2]
                ei += 1
                for kh in range(4):
                    S.dma("pool", wb1[:, kh * 2:(kh + 1) * 2, :],
                          io["exp_w1"][e, kh * 256:(kh + 1) * 256, :].rearrange("(kc k) n -> k kc n", k=128), writes=[wb1])
                for kh in range(2):
                    S.dma("pool", wb2[:, kh * 4:(kh + 1) * 4, :],
                          io["exp_w2"][e, kh * 512:(kh + 1) * 512, :].rearrange("(kc k) n -> k kc n", k=128), writes=[wb2])
                for kc in range(8):
                    S.tt("pool", wb2, wb2[:, kc, :], wb2, wb2[:, kc, :], g2bc, g2bc[:, :], OP.mult)
                for tb in range(QT // 4):
                    at = actT[tb % 2]
                    for hc in range(8):
                        pg, pl_ = S.bank(), S.bank()
                        for kc in range(8):
                            S.mm(pg, pg[:, :], wb1, wb1[:, kc, hc * 256:(hc + 1) * 256:2], h2T, h2T[:, kc, tb * 512:(tb + 1) * 512], kc == 0, kc == 7)
                        for kc in range(8):
                            S.mm(pl_, pl_[:, :], wb1, wb1[:, kc, hc * 256 + 1:(hc + 1) * 256:2], h2T, h2T[:, kc, tb * 512:(tb + 1) * 512], kc == 0, kc == 7)
                        j = hc % 2
                        S.ts("dve", gsb[j], gsb[j][:, :], pg, pg[:, :], b1[:, e, hc, 0:1], 7.0, OP.add, OP.min, extra=[b1])
                        S.act(gss[j], gss[j][:, :], gsb[j], gsb[j][:, :], AF.Gelu_apprx_sigmoid)
                        S.ts("dve", asb[j], asb[j][:, :], pl_, pl_[:, :], b1[:, e, hc, 1:2], -6.0, OP.add, OP.max, extra=[b1])
                        S.stt(at, at[:, hc, :], asb[j], asb[j][:, :], 8.0, gss[j], gss[j][:, :], OP.min, OP.mult)
                    for t4 in range(4):
                        t = tb * 4 + t4
                        tt = qt * QT + t
                        for dh in range(2):
                            py = S.bank()
                            for hc in range(8):
                                S.mm(py, py[:, :], at, at[:, hc, t4 * 128:(t4 + 1) * 128], wb2, wb2[:, hc, dh * 512:(dh + 1) * 512], hc == 0, hc == 7)
                            S.stt(acc, acc[:, t, dh * 512:(dh + 1) * 512], py, py[:, :], gates[:, tt, e:e + 1], acc, acc[:, t, dh * 512:(dh + 1) * 512],
                                  OP.mult, OP.add, extra=[gates])
            for t in range(QT):
                tt = qt * QT + t
                pgt = S.bank()
                S.tr(pgt, pgt[0:32, 0:128], gates, gates[:, tt, :], C["ident"], C["ident"][:, :])
                S.cp("dve", gT, gT[:, :], pgt, pgt[0:32, 0:128])
                for dh in range(2):
                    pb = S.bank()
                    S.mm(pb, pb[:, :], gT, gT[:, :], b2s, b2s[:, dh * 512:(dh + 1) * 512], True, True)
                    S.tt("dve", acc, acc[:, t, dh * 512:(dh + 1) * 512], pb, pb[:, :], acc, acc[:, t, dh * 512:(dh + 1) * 512], OP.add)
                a_ap = acc[:, t, :]
                for i2 in range(2):
                    S.op("dve", (lambda i2, a_ap: lambda e: e.bn_stats(out=stats[:, i2 * 6:(i2 + 1) * 6], in_=a_ap[:, i2 * 512:(i2 + 1) * 512]))(i2, a_ap),
                         reads=[acc], writes=[stats])
                S.op("dve", lambda e: e.bn_aggr(out=mv[:, 0:2], in_=stats[:, 0:12]), reads=[stats], writes=[mv])
                S.act(rstd, rstd[:, 0:1], mv, mv[:, 1:2], AF.Sqrt, bias=C["eps"][:, 0:1], scale=1.0, extra=[C["eps"]])
                S.op("dve", lambda e: e.reciprocal(out=rstd[:, 0:1], in_=rstd[:, 0:1]), reads=[rstd], writes=[rstd])
                S.ts("dve", acc, a_ap, acc, a_ap, mv[:, 0:1], rstd[:, 0:1], OP.subtract, OP.mult, extra=[mv, rstd])
                S.tt("pool", acc, a_ap, acc, a_ap, ln2g, ln2g[:, :], OP.mult)
                S.tt("pool", acc, a_ap, acc, a_ap, ln2b, ln2b[:, :], OP.add)
                S.dma("sp", io["out"][tt * 128:(tt + 1) * 128, :], a_ap, reads=[acc])
        S.barrier()


def ln_mod_stages(S, C, x_ap, xt, xt2, xn, hb, stats, mv, rstd, shiftT, sc1T, dst_ap, dst_buf):
    st_ = {}

    def a1():
        S.dma("sp", xt[0:64, :], x_ap[0:64, :], writes=[xt])
        S.dma("pool", xt[64:128, :], x_ap[64:128, :], writes=[xt2])
        for i in range(2):
            S.op("dve", (lambda i: lambda e: e.bn_stats(out=stats[:, i * 6:(i + 1) * 6], in_=xt[:, i * 512:(i + 1) * 512]))(i),
                 reads=[xt, xt2], writes=[stats])
        S.op("dve", lambda e: e.bn_aggr(out=mv[:, 0:2], in_=stats[:, 0:12]), reads=[stats], writes=[mv])
        S.act(rstd, rstd[:, 0:1], mv, mv[:, 1:2], AF.Sqrt, bias=C["eps"][:, 0:1], scale=1.0, extra=[C["eps"]])

    def a2():
        S.op("dve", lambda e: e.reciprocal(out=rstd[:, 0:1], in_=rstd[:, 0:1]), reads=[rstd], writes=[rstd])
        S.ts("dve", xn, xn[:, :], xt, xt[:, :], mv[:, 0:1], rstd[:, 0:1], OP.subtract, OP.mult, extra=[mv, rstd, xt2])
        st_["pt"] = [S.bank(), S.bank()]
        for kc in range(8):
            p = st_["pt"][kc // 4]
            S.tr(p, p[:, (kc % 4) * 128:(kc % 4 + 1) * 128], xn, xn[:, kc * 128:(kc + 1) * 128], C["ident"], C["ident"][:, :])

    def b_():
        for kc in range(8):
            p = st_["pt"][kc // 4]
            src_ = p[:, (kc % 4) * 128:(kc % 4 + 1) * 128]
            if kc < 4:
                S.act(hb, hb[:, kc, :], p, src_, AF.Identity, bias=shiftT[:, kc:kc + 1], scale=sc1T[:, kc:kc + 1], extra=[C["modsc"]])
            else:
                S.ts("dve", hb, hb[:, kc, :], p, src_, sc1T[:, kc:kc + 1], shiftT[:, kc:kc + 1], OP.mult, OP.add, extra=[C["modsc"]])
        S.dma("act", dst_ap, hb[:, :, :], reads=[hb], writes=[dst_buf])

    return [a1, a2, b_]


class PhaseA0:
    def __init__(self, S, io, C, M, hT_d, hT2_d):
        self.S = S
        modT, modTc = M["modT"], M["modTc"]
        self.ls = ls = ExitStack()
        xt = [S.sb(f"a0xt{i}", [128, 1024], F32, ls) for i in range(4)]
        xt2 = [Buf(f"a0xtH{i}", xt[i].t) for i in range(4)]
        for b_ in xt2:
            ls.callback(S._release, b_)
        xn = [S.sb(f"a0xn{i}", [128, 1024], F32, ls) for i in range(3)]
        hb = [S.sb(f"a0hb{i}", [128, 8, 128], BF16, ls) for i in range(3)]
        stats = [S.sb(f"a0stats{i}", [128, 12], F32, ls) for i in range(3)]
        mv = [S.sb(f"a0mv{i}", [128, 2], F32, ls) for i in range(3)]
        rstd = [S.sb(f"a0rstd{i}", [128, 1], F32, ls) for i in range(3)]
        msc = S.sb("a0msc", [128, 4, 8], F32, ls)
        C["modsc"] = msc
        S.cp("dve", msc, msc[:, 0, :], modT, modT[:, 0, :])
        S.ts("dve", msc, msc[:, 1, :], modT, modT[:, 1, :], 1.0, None, OP.add)
        S.cp("dve", msc, msc[:, 2, :], modTc, modTc[:, 0, :])
        S.ts("dve", msc, msc[:, 3, :], modTc, modTc[:, 1, :], 1.0, None, OP.add)
        jobs = [(io["x_nat"][t * 128:(t + 1) * 128, :], 0, hT_d[t], hT_d) for t in range(64)]
        jobs += [(io["ctx_nat"][t * 128:(t + 1) * 128, :], 2, hT_d[64 + t], hT_d) for t in range(2)]
        self.n_first = len(jobs)
        if hT2_d is not None:
            jobs += [(io["x_perm"][t * 128:(t + 1) * 128, :], 0, hT2_d[t], hT2_d) for t in range(64)]
        self.units = []
        for n, (xap, mi, dap, dbuf) in enumerate(jobs):
            self.units.append(ln_mod_stages(S, C, xap, xt[n % 4], xt2[n % 4], xn[n % 3], hb[n % 3], stats[n % 3], mv[n % 3], rstd[n % 3],
                                            msc[:, mi, :], msc[:, mi + 1, :], dap, dbuf))
        self.slot = 0

    def _emit_slot(self, lo, hi):
        for s_ in (2, 1, 0):
            t_ = self.slot - s_
            if lo <= t_ < hi:
                self.units[t_][s_]()
        self.slot += 1

    def first(self):
        while self.slot < self.n_first + 2:
            self._emit_slot(0, self.n_first)
        self.S.barrier()
        self.slot = self.n_first

    def filler(self, n=1):
        for _ in range(n):
            if self.slot < len(self.units) + 2:
                self._emit_slot(self.n_first, len(self.units))

    def finish(self):
        while self.slot < len(self.units) + 2:
            self._emit_slot(self.n_first, len(self.units))
        self.S.barrier()
        self.ls.close()


def phase_A1(S, io, C, hT_d, ymT_d, filler=lambda: None):
    with ExitStack() as ls:
        Wtok = S.sb("Wtok", [128, 8, 1792], BF16, ls)
        Wfm = S.sb("Wfm", [128, 8, 512], BF16, ls)
        barow = S.sb("barow", [1, 512], F32, ls)
        normg4 = S.sb("normg4", [128, 512], F32, ls)
        Uincl = S.sb("Uincl_sb", [128, 128], F32, ls)
        Lincl = S.sb("Lincl_sb", [128, 128], F32, ls)
        Ustr = S.sb("Ustr_sb", [128, 128], F32, ls)
        Lstr = S.sb("Lstr_sb", [128, 128], F32, ls)
        Mf4 = S.sb("Mf4", [128, 512], F32, ls)
        Mb4 = S.sb("Mb4", [128, 512], F32, ls)
        Sst = {d: S.sb(f"S_{d}", [128, 2, 128], F32, ls) for d in "fb"}
        Sfbf = S.sb("Sfbf", [128, 2, 2, 128], BF16, ls)
        Sbbf = S.sb("Sbbf", [128, 32, 2, 2, 128], BF16, ls)
        hm = S.sb("hm", [128, 2], F32, ls)
        S.memset("dve", hm, hm[0:64, 0:1], 1.0)
        S.memset("dve", hm, hm[64:128, 0:1], 0.0)
        S.memset("dve", hm, hm[0:64, 1:2], 0.0)
        S.memset("dve", hm, hm[64:128, 1:2], 1.0)
        hT = [S.sb(f"a1hT{i}", [128, 8, 128], BF16, ls) for i in range(3)]
        class R2:
            def __init__(self, mk):
                self.bs = [mk(0), mk(1)]
                self.i = 0
            @property
            def c(self):
                return self.bs[self.i % 2]
            def adv(self):
                self.i += 1
        Gs_ = {d: R2(lambda i, d=d: S.sb(f"G_{d}{i}", [128, 256], F32, ls)) for d in "fb"}
        e1_ = R2(lambda i: S.sb(f"e1{i}", [128, 256], F32, ls))
        Ekh_ = R2(lambda i: S.sb(f"Ekh{i}", [128, 256], F32, ls))
        khat_ = R2(lambda i: S.sb(f"khat{i}", [128, 256], BF16, ls))
        vbf_ = R2(lambda i: S.sb(f"vbf{i}", [128, 512], BF16, ls))
        etot_ = R2(lambda i: S.sb(f"etot{i}", [128, 4], F32, ls))
        Eq_ = R2(lambda i: S.sb(f"Eq{i}", [128, 512], F32, ls))
        Ek_ = R2(lambda i: S.sb(f"Ek{i}", [128, 512], F32, ls))
        qtl_ = R2(lambda i: S.sb(f"qtl{i}", [128, 512], BF16, ls))
        ktl_ = R2(lambda i: S.sb(f"ktl{i}", [128, 2, 512], BF16, ls))
        Af_ = R2(lambda i: S.sb(f"Af{i}", [128, 512], BF16, ls))
        Ab_ = R2(lambda i: S.sb(f"Ab{i}", [128, 512], BF16, ls))
        ssq_ = R2(lambda i: S.sb(f"ssq{i}", [128, 4], F32, ls))
        junk = S.sb("junk", [128, 128], F32, ls)
        on_ = R2(lambda i: S.sb(f"on{i}", [128, 512], F32, ls))
        sg_ = R2(lambda i: S.sb(f"sg{i}", [128, 512], F32, ls))
        rings = list(Gs_.values()) + [e1_, Ekh_, khat_, vbf_, etot_, Eq_, Ek_, qtl_, ktl_, Af_, Ab_, ssq_, on_, sg_]

        def adv_all():
            for r_ in rings:
                r_.adv()
        ymt = [S.sb(f"ymt{i}", [128, 512], BF16, ls) for i in range(2)]
        one1 = C["ones"]
        with ExitStack() as l2:
            aT = {d: S.sb(f"aT_{d}", [16, 1024], F32, l2) for d in "fb"}
            wa = {d: S.sb(f"wa_{d}", [16, 256], F32, l2) for d in "fb"}
            rowb = S.sb("a1rowb", [1, 512], F32, l2)
            for nm, dst, c0, wdt in (("w_k", Wtok, 0, 256), ("w_v", Wtok, 256, 512), ("w_g", Wtok, 768, 512)):
                S.dma("pool", dst[:, :, c0:c0 + wdt], io[nm].rearrange("(kc k) n -> k kc n", k=128), writes=[dst])
            S.dma("pool", Wfm[:, :, 0:256], io["w_q"].rearrange("(kc k) n -> k kc n", k=128), writes=[Wfm])
            S.dma("pool", Wfm[:, :, 256:512], io["w_k"].rearrange("(kc k) n -> k kc n", k=128), writes=[Wfm])
            S.ts("dve", Wfm, Wfm[:, :, 0:256], Wfm, Wfm[:, :, 0:256], 0.125, None, OP.mult)
            for d in "fb":
                S.dma("sp", aT[d][:, :], io[f"aT_{d}"], writes=[aT[d]])
                S.dma("sp", wa[d][:, :], io[f"wa_{d}"], writes=[wa[d]])
            for di, d in enumerate("fb"):
                for kc in range(8):
                    pb = S.bank()
                    S.mm(pb, pb[:, 0:256], aT[d], aT[d][:, kc * 128:(kc + 1) * 128], wa[d], wa[d][:, :], True, True)
                    S.cp("dve", Wtok, Wtok[:, kc, 1280 + di * 256:1536 + di * 256], pb, pb[:, 0:256])
            S.dma("sp", barow[:, :], io["ba"], writes=[barow])
            for nm, dst in (("Uincl", Uincl), ("Lincl", Lincl), ("Ustr", Ustr), ("Lstr", Lstr)):
                S.dma("sp", dst[:, :], io[nm], writes=[dst])
            for h in range(4):
                S.cp("dve", Mf4, Mf4[:, h * 128:(h + 1) * 128], Uincl, Uincl[:, :])
                S.cp("dve", Mb4, Mb4[:, h * 128:(h + 1) * 128], Lincl, Lincl[:, :])
            S.dma("sp", rowb[0:1, 0:128], io["normg"], writes=[rowb])
            pb = S.bank()
            S.mm(pb, pb[:, 0:128], C["ones"], C["ones"][0:1, 0:128], rowb, rowb[0:1, 0:128], True, True)
            for h in range(4):
                S.cp("dve", normg4, normg4[:, h * 128:(h + 1) * 128], pb, pb[:, 0:128])
            S.barrier()
        for d in "fb":
            S.memset("dve", Sst[d], Sst[d][:, :, :], 0.0)
        hti = [0]
        if DEBUG.get("stop") == "a1prep":
            S.barrier()
            return

        def load_hT(t):
            b = hT[hti[0] % 3]
            hti[0] += 1
            S.dma("sp", b[:, :, :], hT_d[t], writes=[b], reads=[hT_d])
            return b

        def proj_tok(hb, c0, wdt, bias_c0=None):
            pb = S.bank()
            for kc in range(8):
                S.mm(pb, pb[:, 0:wdt], hb, hb[:, kc, :], Wtok, Wtok[:, kc, c0:c0 + wdt], kc == 0, (kc == 7) and bias_c0 is None)
            if bias_c0 is not None:
                S.mm(pb, pb[:, 0:wdt], C["ones"], C["ones"][0:1, 0:128], barow, barow[0:1, bias_c0:bias_c0 + wdt], False, True)
            return pb

        def make_G(d, pz):
            S.act(e1_.c, e1_.c[:, :], pz, pz[:, 0:256], AF.Exp, scale=-1.0)
            S.act(Gs_[d].c, Gs_[d].c[:, :], e1_.c, e1_.c[:, :], AF.Ln, bias=one1[:, 0:1], scale=1.0, extra=[one1])

        def su_prep(d, pk):
            strict = Lstr if d == "f" else Ustr
            pD = S.bank()
            S.mm(pD, pD[:, 0:256], strict, strict[:, :], Gs_[d].c, Gs_[d].c[:, :], True, True)
            S.act(Ekh_.c, Ekh_.c[:, :], pD, pD[:, 0:256], AF.Exp, scale=-1.0 / 16)
            S.tt("dve", khat_.c, khat_.c[:, :], pk, pk[:, 0:256], Ekh_.c, Ekh_.c[:, :], OP.mult)
            pT = S.bank()
            for p in range(2):
                S.mm(pT, pT[:, p * 2:p * 2 + 2], Gs_[d].c, Gs_[d].c[:, p * 128:(p + 1) * 128], C["ones"], C["ones"][:, 0:2], True, True)
            S.act(etot_.c, etot_.c[:, :], pT, pT[:, 0:4], AF.Exp, scale=-1.0 / 16)

        def su_apply(d, khat=None, vbf=None, etot=None):
            khat = khat or khat_.c
            vbf = vbf or vbf_.c
            etot = etot or etot_.c
            pu = S.bank()
            for h in range(4):
                p, s = h // 2, h % 2
                S.mm(pu, pu[s * 64:(s + 1) * 64, p * 128:(p + 1) * 128], khat, khat[:, h * 64:(h + 1) * 64], vbf, vbf[:, h * 128:(h + 1) * 128], True, True)
            for p in range(2):
                S.stt(Sst[d], Sst[d][:, p, :], Sst[d], Sst[d][:, p, :], etot[:, p * 2:p * 2 + 1], pu, pu[:, p * 128:(p + 1) * 128], OP.mult, OP.add, extra=[etot])

        def state_only(d, t):
            adv_all()
            filler()
            hb = load_hT(t)
            pk = proj_tok(hb, 0, 256)
            pv = proj_tok(hb, 256, 512)
            pz = proj_tok(hb, 1280 if d == "f" else 1536, 256, bias_c0=0 if d == "f" else 256)
            S.cp("act", vbf_.c, vbf_.c[:, :], pv, pv[:, :])
            make_G(d, pz)
            su_prep(d, pk)
            su_apply(d)

        state_only("f", 64); state_only("f", 65)
        state_only("b", 65); state_only("b", 64)
        if DEBUG.get("stop") == "a1ctx":
            S.barrier()
            return
        for n in range(63, -1, -1):
            if n < 32:
                for s in range(2):
                    S.ts("dve", Sbbf, Sbbf[:, n, :, s, :], Sst["b"], Sst["b"][:, :, :], hm[:, s:s + 1], None, OP.mult, extra=[hm])
            if n > 0:
                state_only("b", n)
        if DEBUG.get("stop") == "a1bwd":
            S.barrier()
            return
        def fwd_chunk(n):
            adv_all()
            hb = load_hT(n)
            vbf, sg, Eq, Ek, qtl, ktl, Af, Ab, ssq, on = vbf_.c, sg_.c, Eq_.c, Ek_.c, qtl_.c, ktl_.c, Af_.c, Ab_.c, ssq_.c, on_.c
            khat, etot = khat_.c, etot_.c
            pq = S.bank()
            for j in range(4):
                for kc in range(8):
                    S.mm(pq, pq[:, j * 128:(j + 1) * 128], Wfm, Wfm[:, kc, j * 128:(j + 1) * 128], hb, hb[:, kc, :], kc == 0, kc == 7)
            pk = proj_tok(hb, 0, 256)
            pv = proj_tok(hb, 256, 512)
            pg = proj_tok(hb, 768, 512)
            pzf = proj_tok(hb, 1280, 256, bias_c0=0)
            pzb = proj_tok(hb, 1536, 256, bias_c0=256)
            S.cp("act", vbf, vbf[:, :], pv, pv[:, :])
            S.act(sg, sg[:, :], pg, pg[:, :], AF.Silu)
            make_G("f", pzf)
            make_G("b", pzb)
            pc = S.bank()
            for di, d in enumerate("fb"):
                tri = Uincl if d == "f" else Lincl
                for p in range(2):
                    j = di * 2 + p
                    S.mm(pc, pc[:, j * 128:(j + 1) * 128], Gs_[d].c, Gs_[d].c[:, p * 128:(p + 1) * 128], tri, tri[:, :], True, True)
            S.act(Eq, Eq[:, :], pc, pc[:, :], AF.Exp, scale=-1.0 / 16)
            S.act(Ek, Ek[:, :], pc, pc[:, :], AF.Exp, scale=1.0 / 16)
            for di in range(2):
                S.tt("dve", qtl, qtl[:, di * 256:(di + 1) * 256], pq, pq[:, 0:256], Eq, Eq[:, di * 256:(di + 1) * 256], OP.mult)
                for s in range(2):
                    S.stt(ktl, ktl[:, s, di * 256:(di + 1) * 256], pq, pq[:, 256:512], hm[:, s:s + 1], Ek, Ek[:, di * 256:(di + 1) * 256],
                          OP.mult, OP.mult, extra=[hm])
            su_prep("f", pk)
            for di, (A, Mk) in enumerate(((Af, Mf4), (Ab, Mb4))):
                psc = S.bank()
                for h in range(4):
                    p, s = h // 2, h % 2
                    j = di * 2 + p
                    S.mm(psc, psc[:, h * 128:(h + 1) * 128], ktl, ktl[:, s, j * 128:(j + 1) * 128],
                         qtl, qtl[:, j * 128:(j + 1) * 128], True, True)
                S.tt("dve", A, A[:, :], psc, psc[:, :], Mk, Mk[:, :], OP.mult)

            def O():
                for s in range(2):
                    S.ts("dve", Sfbf, Sfbf[:, :, s, :], Sst["f"], Sst["f"][:, :, :], hm[:, s:s + 1], None, OP.mult, extra=[hm])
                po = S.bank()
                for h in range(4):
                    p, s = h // 2, h % 2
                    oap = po[:, h * 128:(h + 1) * 128]
                    S.mm(po, oap, Af, Af[:, h * 128:(h + 1) * 128], vbf, vbf[:, h * 128:(h + 1) * 128], True, False)
                    S.mm(po, oap, Ab, Ab[:, h * 128:(h + 1) * 128], vbf, vbf[:, h * 128:(h + 1) * 128], False, False)
                    S.mm(po, oap, qtl, qtl[:, p * 128:(p + 1) * 128], Sfbf, Sfbf[:, p, s, :], False, False)
                    S.mm(po, oap, qtl, qtl[:, (2 + p) * 128:(3 + p) * 128], Sbbf, Sbbf[:, n, p, s, :], False, True)
                su_apply("f", khat, vbf, etot)
                for h in range(4):
                    S.act(junk, junk[:, :], po, po[:, h * 128:(h + 1) * 128], AF.Square, accum=ssq[:, h:h + 1], accb=ssq)
                S.act(ssq, ssq[:, :], ssq, ssq[:, :], AF.Sqrt, bias=C["eps"][:, 0:1], scale=1.0 / 128, extra=[C["eps"]])
                S.op("dve", lambda e: e.reciprocal(out=ssq[:, :], in_=ssq[:, :]), reads=[ssq], writes=[ssq])
                for h in range(4):
                    S.ts("dve", on, on[:, h * 128:(h + 1) * 128], po, po[:, h * 128:(h + 1) * 128], ssq[:, h:h + 1], None, OP.mult, extra=[ssq])
                S.tt("pool", on, on[:, :], on, on[:, :], normg4, normg4[:, :], OP.mult)
                S.tt("dve", on, on[:, :], on, on[:, :], sg, sg[:, :], OP.mult)
                pt = S.bank()
                for j in range(4):
                    S.tr(pt, pt[:, j * 128:(j + 1) * 128], on, on[:, j * 128:(j + 1) * 128], C["ident"], C["ident"][:, :])
                y = ymt[n % 2]
                S.cp("act", y, y[:, :], pt, pt[:, :])
                S.dma("sp", ymT_d[n][:, 0:4, :], y[:, :].rearrange("p (a b) -> p a b", a=4), reads=[y], writes=[ymT_d])

            return O

        pend = None
        for n in range(DEBUG.get("nfwd", 32)):
            o_ = fwd_chunk(n)
            if pend is not None:
                pend()
            pend = o_
        pend()
        S.barrier()


HY_L = 8192
HY_N = 16384
HY_DELTAS = np.abs(np.linspace(np.log(1e-2) / 1.5, np.log(1e-2) / 0.3, 512, dtype=np.float32)).astype(np.float64)


def hyena_tables():
    import ml_dtypes
    bf = ml_dtypes.bfloat16
    pi_idx = np.arange(128)
    tlo = 2 * (pi_idx % 64) + pi_idx // 64
    k = np.arange(128)
    T = {}
    th = np.arange(128)
    ph = 2 * np.pi * np.outer(th, k) / 128.0
    F1f = np.concatenate([np.cos(ph), -np.sin(ph)], 1)
    T["F1f"] = F1f.astype(bf)
    T["F1d"] = F1f[np.arange(128) % 64].astype(bf)
    tw = 2 * np.pi * np.outer(tlo, k) / HY_N
    c, s = np.cos(tw), np.sin(tw)
    T["TWc4"] = np.tile(np.concatenate([c, c], 1)[:, None, :], (1, 4, 1)).reshape(128, 1024).astype(bf)
    T["TWs4"] = np.tile(np.concatenate([s, s], 1)[:, None, :], (1, 4, 1)).reshape(128, 1024).astype(bf)
    c2, s2 = c.T, s.T
    T["TW2c4"] = np.tile(np.concatenate([c2, c2], 1)[:, None, :], (1, 4, 1)).reshape(128, 1024).astype(bf)
    T["TW2s4"] = np.tile(np.concatenate([s2, s2], 1)[:, None, :], (1, 4, 1)).reshape(128, 1024).astype(bf)
    p2 = 2 * np.pi * np.outer(tlo, k) / 128.0
    T["C2"] = np.cos(p2).astype(bf); T["S2"] = np.sin(p2).astype(bf); T["nS2"] = (-np.sin(p2)).astype(bf); T["nC2"] = (-np.cos(p2)).astype(bf)
    p3 = p2.T
    T["R3a"] = np.concatenate([np.cos(p3), np.sin(p3)], 1).astype(bf)
    T["R3b"] = np.concatenate([-np.sin(p3), np.cos(p3)], 1).astype(bf)
    T["nR3a"] = (-np.concatenate([np.cos(p3), np.sin(p3)], 1)).astype(bf)
    p4 = 2 * np.pi * np.outer(k, np.arange(64)) / 128.0
    T["C4"] = np.cos(p4).astype(bf); T["nS4"] = (-np.sin(p4)).astype(bf); T["nC4"] = (-np.cos(p4)).astype(bf)
    tau = 128 * th[:, None] + tlo[None, :]
    lag = np.where(tau < HY_L, tau, HY_N - tau)
    lag = np.where(tau == HY_L, 0, lag)
    T["tnl"] = (lag / (HY_L - 1.0)).astype(np.float32)
    lagq = lag.T.reshape(-1).astype(np.float64)
    bands = 16
    f = np.linspace(1e-4, bands - 1, bands)
    ang = (2.0 * np.pi * lagq / HY_L)[:, None] * f[None, :]
    z = np.concatenate([(lagq / (HY_L - 1.0))[:, None], np.cos(ang), -np.sin(ang)], -1)
    T["zT"] = np.ascontiguousarray(z.T).astype(np.float32)
    return T


def phase_A2(S, io, C, hT2_d, ymT_d):
    PI = float(np.pi)
    with ExitStack() as ls:
        tb = {}
        for nm, shp in (("F1f", [128, 256]), ("F1d", [128, 256]), ("TWc4", [128, 1024]), ("TWs4", [128, 1024]), ("TW2c4", [128, 1024]),
                        ("TW2s4", [128, 1024]), ("C2", [128, 128]), ("S2", [128, 128]), ("nS2", [128, 128]), ("R3a", [128, 256]),
                        ("R3b", [128, 256]), ("C4", [128, 64]), ("nS4", [128, 64]), ("nC2", [128, 128]), ("nR3a", [128, 256]), ("nC4", [128, 64])):
            tb[nm] = S.sb("t_" + nm, shp, BF16, ls)
            S.dma("sp", tb[nm][:, :], io[nm], writes=[tb[nm]])
        tnl = S.sb("tnl_sb", [128, 128], F32, ls)
        S.dma("sp", tnl[:, :], io["tnl"], writes=[tnl])
        hid2T = S.sb("hid2T", [64, HY_N], BF16, ls)
        with ExitStack() as l2:
            w1 = S.sb("fw1", [33, 64], F32, l2)
            w2 = S.sb("fw2", [64, 64], F32, l2)
            fb = S.sb("ffb", [64, 4], F32, l2)
            zt = [S.sb(f"fzt{i}", [33, 512], F32, l2) for i in range(2)]
            pre = [S.sb(f"fpre{i}", [64, 512], F32, l2) for i in range(2)]
            tmp = S.sb("ftmp", [64, 512], F32, l2)
            h1 = S.sb("fh1", [64, 512], F32, l2)
            S.dma("sp", w1[:, :], io["hy_w1"], writes=[w1])
            S.dma("sp", w2[:, :], io["hy_w2"], writes=[w2])
            S.dma("sp", fb[:, 0:3], io["hy_fb"], writes=[fb])
            S.tt("dve", fb, fb[:, 0:1], fb, fb[:, 0:1], fb, fb[:, 2:3], OP.mult)
            S.tt("dve", fb, fb[:, 1:2], fb, fb[:, 1:2], fb, fb[:, 2:3], OP.mult)

            def sin_layer(ps, bcol, out_b, out_ap, k):
                p = pre[k % 2]
                S.ts("dve", p, p[:, :], ps, ps[0:64, :], fb[:, 2:3], fb[:, bcol:bcol + 1], OP.mult, OP.add, extra=[fb])
                S.ts("dve", tmp, tmp[:, :], p, p[:, :], PI, -2 * PI, OP.is_gt, OP.mult)
                S.tt("dve", p, p[:, :], p, p[:, :], tmp, tmp[:, :], OP.add)
                S.ts("dve", tmp, tmp[:, :], p, p[:, :], -PI, 2 * PI, OP.is_lt, OP.mult)
                S.tt("dve", p, p[:, :], p, p[:, :], tmp, tmp[:, :], OP.add)
                S.act(out_b, out_ap, p, p[:, :], AF.Sin)

            for blk in range(HY_N // 512):
                z = zt[blk % 2]
                S.dma("sp", z[:, :], io["zT"][:, blk * 512:(blk + 1) * 512], writes=[z])
                ps = S.bank()
                S.mm(ps, ps[0:64, :], w1, w1[:, :], z, z[:, :], True, True)
                sin_layer(ps, 0, h1, h1[:, :], blk)
                ps2 = S.bank()
                S.mm(ps2, ps2[0:64, :], w2, w2[:, :], h1, h1[:, :], True, True)
                sin_layer(ps2, 1, hid2T, hid2T[:, blk * 512:(blk + 1) * 512], blk + 1)
            S.barrier()
        UX = S.sb("UX", [128, 64, 256], BF16, ls)
        X2 = S.sb("X2", [128, 64, 128], BF16, ls)
        Z = S.sb("Zc", [128, 64, 128], BF16, ls)
        Y2 = S.sb("Y2", [128, 64, 128], BF16, ls)
        identb = S.sb("identb", [128, 128], BF16, ls)
        S.cp("dve", identb, identb[:, :], C["ident"], C["ident"][:, :])
        S.memset("dve", Y2, Y2[:, :, :], 0.0)
        for ps_i in range(DEBUG.get("a2_passes", 4)):
            c0 = ps_i * 128
            with ExitStack() as l2:
                Wr = S.sb("hWr", [128, 8, 384], BF16, l2)
                Wt = [S.sb(f"hWt{i}", [128, 8, 384], BF16, l2) for i in range(3)]
                cwb = [S.sb(f"hcw{i}", [128, 384], F32, l2) for i in range(3)]
                cbb = S.sb("hcb", [128, 384], F32, l2)
                rowb = S.sb("hrowb", [1, 512], F32, l2)
                ring = [S.sb(f"hring{i}", [128, 8, 128], BF16, l2) for i in range(4)]
                S.dma("pool", Wr[:, :, :], io["w_hy_p"][ps_i].rearrange("(kc k) n -> k kc n", k=128), writes=[Wr])
                for i in range(3):
                    bcast_row(S, C, cwb[i], io["cw_p"][ps_i, i:i + 1, :], rowb, 384)
                    for kc in range(8):
                        S.tt("pool", Wt[i], Wt[i][:, kc, :], Wr, Wr[:, kc, :], cwb[i], cwb[i][:, :], OP.mult)
                bcast_row(S, C, cbb, io["cb_p"][ps_i], rowb, 384)
                tiles = {}
                shr = [S.sb(f"hshr{i}", [128, 8, 128], BF16, l2) for i in range(3)]
                shs = {}

                def get_tile(j):
                    if j not in tiles:
                        b = ring[j % 4]
                        S.dma("sp", b[:, :, :], hT2_d[j], writes=[b], reads=[hT2_d])
                        tiles[j] = b
                    return tiles[j]

                def get_sh(j):
                    if j not in shs:
                        b = shr[j % 3]
                        a_, c_ = get_tile(j - 1), get_tile(j)
                        S.cp("pool", b, b[:, :, 0:64], a_, a_[:, :, 64:128])
                        S.cp("pool", b, b[:, :, 64:128], c_, c_[:, :, 0:64])
                        shs[j] = b
                    return shs[j]

                for j in range(64):
                    cur = get_tile(j)
                    shl = get_sh(j) if j not in (0, 32) else None
                    shrt = get_sh(j + 1) if j not in (31, 63) else None
                    pb = S.bank()
                    mms = []
                    for kc in range(8):
                        mms.append((pb[:, 0:384], cur, cur[:, kc, :], Wt[1], Wt[1][:, kc, :]))
                        if shl is not None:
                            mms.append((pb[:, 0:384], shl, shl[:, kc, :], Wt[0], Wt[0][:, kc, :]))
                        else:
                            mms.append((pb[64:128, 0:384], cur, cur[:, kc, 0:64], Wt[0], Wt[0][:, kc, :]))
                        if shrt is not None:
                            mms.append((pb[:, 0:384], shrt, shrt[:, kc, :], Wt[2], Wt[2][:, kc, :]))
                        else:
                            mms.append((pb[0:64, 0:384], cur, cur[:, kc, 64:128], Wt[2], Wt[2][:, kc, :]))
                    last_center = [m_ for m_ in mms if m_[1] is cur and m_[3] is Wt[1]][-1]
                    mms.remove(last_center)
                    mms.append(last_center)
                    for mi, (o, lb, lap, rb, rap) in enumerate(mms):
                        S.mm(pb, o, lb, lap, rb, rap, mi == 0, mi == len(mms) - 1)
                    S.tt("dve", UX, UX[:, j, :], pb, pb[:, 0:256], cbb, cbb[:, 0:256], OP.add)
                    S.tt("dve", X2, X2[:, j, :], pb, pb[:, 256:384], cbb, cbb[:, 256:384], OP.add)
                S.barrier()
            with ExitStack() as l2:
                wo = S.sb("hwo", [64, 3, 2, 128], BF16, l2)
                dbn = S.sb("hdbn", [128, 256], F32, l2)
                rowb = S.sb("hrowb2", [1, 256], F32, l2)
                TS = S.sb("hTS", [128, 2, 16, 128], BF16, l2)
                Wn = S.sb("hWn", [128, 16, 128], BF16, l2)
                Hs = [S.sb(f"hH{i}", [128, 2, 8, 256], BF16, l2) for i in range(2)]
                CB = [[S.sb(f"hcb{c}_{i}", [128, 1024], BF16, l2) for i in range(3)] for c in range(4)]
                S.dma("pool", wo[:, :, :, :], io["wo_p"][ps_i], writes=[wo])
                bcast_row(S, C, dbn, io["db_p"][ps_i], rowb, 256)
                S.ts("dve", dbn, dbn[:, :], dbn, dbn[:, :], 1.0 / HY_N, None, OP.mult)
                NG = DEBUG.get("a2_groups", 16)

                def v4(bf):
                    return bf[:, :].rearrange("p (c r k) -> p c r k", c=4, r=2)

                def x4(bf):
                    return bf[:, :].rearrange("p (r c k) -> p r c k", r=2, c=4)

                def fwd_stages(st_, lhs_list, F1, K, bufs, pool_share=False):
                    b0, b1, b2 = bufs

                    def s0():
                        st_["pa"] = [S.bank(), S.bank()]
                        for ch in range(4):
                            p = st_["pa"][ch // 2]
                            for (lb, lap, psl) in lhs_list[ch]:
                                S.mm(p, p[psl, (ch % 2) * 256:(ch % 2 + 1) * 256], lb, lap, F1, F1[psl, :] if K == 64 else F1[:, :], True, True)

                    def s1():
                        if pool_share:
                            for h in range(2):
                                S.cp("act", b0, b0[:, h * 512:(h + 1) * 512], st_["pa"][h], st_["pa"][h][:, :])

                    def s2():
                        for h in range(2):
                            pa_ = st_["pa"][h]
                            cs = slice(h * 512, (h + 1) * 512)
                            S.tt("dve", b1, b1[:, cs], pa_, pa_[:, :], tb["TWc4"], tb["TWc4"][:, cs], OP.mult)
                            if not pool_share:
                                S.tt("dve", b2, b2[:, cs], pa_, pa_[:, :], tb["TWs4"], tb["TWs4"][:, cs], OP.mult)
                        if pool_share:
                            S.tt("pool", b2, b2[:, :], b0, b0[:, :], tb["TWs4"], tb["TWs4"][:, :], OP.mult)

                    def s3():
                        pr, pi_ = S.bank(), S.bank()
                        p1, p2 = v4(b1), v4(b2)
                        terms_r = ((tb["C2"], b1, p1[:, :, 0, :]), (tb["C2"], b2, p2[:, :, 1, :]), (tb["S2"], b1, p1[:, :, 1, :]), (tb["nS2"], b2, p2[:, :, 0, :]))
                        terms_i = ((tb["C2"], b1, p1[:, :, 1, :]), (tb["nC2"], b2, p2[:, :, 0, :]), (tb["nS2"], b1, p1[:, :, 0, :]), (tb["nS2"], b2, p2[:, :, 1, :]))
                        for po_, terms in ((pr, terms_r), (pi_, terms_i)):
                            for ti, (tbl, bb, ap_) in enumerate(terms):
                                S.mm(po_, po_[:, :], tbl, tbl[:, :], bb, ap_, ti == 0, ti == 3)
                        st_["pr"], st_["pi"] = pr, pi_

                    return [s0, s1, s2, s3]

                def filt_chain(g, o_, half, bufs):
                    H = Hs[g % 2]
                    st_ = {}
                    lhs = [[(TS, TS[:, o_, (g % 2) * 8 + half * 4 + ch, :], slice(0, 128))] for ch in range(4)]
                    stages = fwd_stages(st_, lhs, tb["F1f"], 128, bufs)

                    def s5():
                        pr, pi_ = st_["pr"], st_["pi"]
                        for ch in range(4):
                            cidx = o_ * 128 + g * 8 + half * 4 + ch
                            S.act(H, H[:, o_, half * 4 + ch, 0:128], pr, pr[:, ch * 128:(ch + 1) * 128], AF.Identity,
                                  bias=dbn[:, cidx:cidx + 1], scale=1.0 / HY_N, extra=[dbn])
                        S.act(H, H[:, o_, half * 4:(half + 1) * 4, 128:256], pi_, pi_[:, :].rearrange("p (c k) -> p c k", c=4), AF.Identity, scale=1.0 / HY_N)

                    return stages + [s5]

                def conv_chain(g, order, half, src, src_c0, bufs):
                    H = Hs[g % 2]
                    b0, b1, b2 = bufs
                    st_ = {}
                    lhs = []
                    for ch in range(4):
                        cc = src_c0 + g * 8 + half * 4 + ch
                        lhs.append([(src, src[0:64, :, cc], slice(0, 64)), (src, src[64:128, :, cc], slice(64, 128))])
                    stages = fwd_stages(st_, lhs, tb["F1d"], 64, bufs, pool_share=False)
                    Hv = H[:, order, half * 4:(half + 1) * 4, :].rearrange("p c (r k) -> p r c k", r=2)

                    def s5():
                        S.cp("act", b0, x4(b0)[:, 0, :, :], st_["pr"], st_["pr"][:, :].rearrange("p (c k) -> p c k", c=4))
                        S.cp("act", b0, x4(b0)[:, 1, :, :], st_["pi"], st_["pi"][:, :].rearrange("p (c k) -> p c k", c=4))

                    def s6():
                        S.tt("dve", b1, x4(b1)[:, 0, :, :], st_["pr"], st_["pr"][:, :].rearrange("p (c k) -> p c k", c=4), H, Hv[:, 0, :, :], OP.mult)
                        S.tt("dve", b1, x4(b1)[:, 1, :, :], st_["pi"], st_["pi"][:, :].rearrange("p (c k) -> p c k", c=4), H, Hv[:, 0, :, :], OP.mult)
                        for r in range(2):
                            S.tt("pool", b2, x4(b2)[:, r, :, :], b0, x4(b0)[:, r, :, :], H, Hv[:, 1, :, :], OP.mult)

                    def s7():
                        st_["pb3"] = [S.bank(), S.bank()]
                        m1, m2 = x4(b1), x4(b2)
                        for ch in range(4):
                            p = st_["pb3"][ch // 2]
                            o = p[:, (ch % 2) * 256:(ch % 2 + 1) * 256]
                            S.mm(p, o, b1, m1[:, 0, ch, :], tb["R3a"], tb["R3a"][:, :], True, False)
                            S.mm(p, o, b2, m2[:, 1, ch, :], tb["nR3a"], tb["nR3a"][:, :], False, False)
                            S.mm(p, o, b2, m2[:, 0, ch, :], tb["R3b"], tb["R3b"][:, :], False, False)
                            S.mm(p, o, b1, m1[:, 1, ch, :], tb["R3b"], tb["R3b"][:, :], False, True)

                    def s8():
                        for h in range(2):
                            S.cp("act", b0, b0[:, h * 512:(h + 1) * 512], st_["pb3"][h], st_["pb3"][h][:, :])

                    def s9():
                        for h in range(2):
                            pb_ = st_["pb3"][h]
                            cs = slice(h * 512, (h + 1) * 512)
                            S.tt("dve", b1, b1[:, cs], pb_, pb_[:, :], tb["TW2c4"], tb["TW2c4"][:, cs], OP.mult)
                        S.tt("pool", b2, b2[:, :], b0, b0[:, :], tb["TW2s4"], tb["TW2s4"][:, :], OP.mult)

                    return stages + [s5, s6, s7, s8, s9]

                def conv_tail(g, cbs, gate, gate_c0, dst, mrows):
                    p4 = S.bank()
                    for half in range(2):
                        b1, b2 = cbs[half][1], cbs[half][2]
                        q1, q2 = v4(b1), v4(b2)
                        for par in range(2):
                            o = p4[par * 64:par * 64 + mrows, half * 256:(half + 1) * 256]
                            cs = slice(par * 64, (par + 1) * 64)
                            S.mm(p4, o, tb["C4"], tb["C4"][:, 0:mrows], b1, q1[:, :, 0, cs], True, False)
                            S.mm(p4, o, tb["nC4"], tb["nC4"][:, 0:mrows], b2, q2[:, :, 1, cs], False, False)
                            S.mm(p4, o, tb["nS4"], tb["nS4"][:, 0:mrows], b2, q2[:, :, 0, cs], False, False)
                            S.mm(p4, o, tb["nS4"], tb["nS4"][:, 0:mrows], b1, q1[:, :, 1, cs], False, True)
                    for par in range(2):
                        rows = slice(par * 64, par * 64 + mrows)
                        gv = gate[rows, :, gate_c0 + g * 8:gate_c0 + g * 8 + 8].rearrange("p j c -> p c j")
                        dv = dst[rows, :, g * 8:g * 8 + 8].rearrange("p j c -> p c j")
                        S.tt("dve", dst, dv, p4, p4[rows, :].rearrange("p (c j) -> p c j", c=8), gate, gv, OP.mult)

                def ts_gen(g2):
                    for ch in range(16):
                        S.act(Wn, Wn[:, ch, :], tnl, tnl[:, :], AF.Exp, scale=-float(HY_DELTAS[c0 + g2 * 16 + ch]))
                    for blk in range(8):
                        p = S.bank()
                        for i_ in range(16):
                            pi_i = blk * 16 + i_
                            col = i_ * 32
                            S.mm(p, p[0:64, col:col + 32], hid2T, hid2T[:, pi_i * 128:pi_i * 128 + 64], wo, wo[:, 0, :, g2 * 16:(g2 + 1) * 16], True, True)
                            S.mm(p, p[64:128, col:col + 32], hid2T, hid2T[:, pi_i * 128 + 64:pi_i * 128 + 128], wo, wo[:, 1, :, g2 * 16:(g2 + 1) * 16], True, True)
                        for o_ in range(2):
                            src_v = p[:, :].rearrange("p (i o c) -> p o c i", i=16, o=2)[:, o_, :, :]
                            S.tt("dve", TS, TS[:, o_, :, blk * 16:(blk + 1) * 16], p, src_v, Wn, Wn[:, :, blk * 16:(blk + 1) * 16], OP.mult)
                    S.memset("dve", TS, TS[64:65, :, :, 0:1], 0.0)
                    pl0 = S.bank()
                    S.mm(pl0, pl0[0:1, 0:32], hid2T, hid2T[:, 0:1], wo, wo[:, 2, :, g2 * 16:(g2 + 1) * 16], True, True)
                    S.cp("dve", TS, TS[0:1, :, :, 0:1], pl0, pl0[0:1, 0:32].rearrange("p (a b c) -> p a b c", a=2, c=1))

                def interleave(chains):
                    for s_ in range(max(len(c_) for c_ in chains)):
                        for c_ in chains:
                            if s_ < len(c_):
                                c_[s_]()

                ts_gen(0)
                interleave([filt_chain(0, o_, half, CB[o_ * 2 + half]) for o_ in range(2) for half in range(2)])
                for g in range(NG):
                    nx = g + 1 < NG
                    if nx and (g + 1) % 2 == 0:
                        ts_gen((g + 1) // 2)
                    for order in range(2):
                        if order == 0:
                            chains = [conv_chain(g, 0, half, UX, 0, CB[half]) for half in range(2)]
                        else:
                            chains = [conv_chain(g, 1, half, Z, 0, CB[half]) for half in range(2)]
                        if nx:
                            chains += [filt_chain(g + 1, order, half, CB[2 + half]) for half in range(2)]
                        interleave(chains)
                        if order == 0:
                            conv_tail(g, CB, UX, 128, Z, 64)
                        else:
                            conv_tail(g, CB, X2, 0, Y2, 32)
                S.barrier()
            with ExitStack() as l2:
                Yb = S.sb("hYb", [128, 32, 128], BF16, l2)
                for j in range(64):
                    pt = S.bank()
                    ptb = pt[:, 0:64].bitcast(BF16)
                    S.tr(pt, ptb, Y2, Y2[:, j, :], identb, identb[:, :])
                    S.cp("act" if j % 2 else "dve", Yb, Yb[:, :, 2 * j:2 * j + 2].rearrange("c t p -> c p t"),
                         pt, ptb.rearrange("c (p t) -> c p t", p=2)[:, :, 0:32])
                for n in range(32):
                    S.dma("sp", ymT_d[n][:, 4 + ps_i, :], Yb[:, n, :], reads=[Yb], writes=[ymT_d])
                S.barrier()


def build(mode="full", NT=32):
    nc = bass.Bass("TRN2", target_bir_lowering=False)
    st = ExitStack()
    with st:
        S = Sched(nc, st)
        S.banks = [S.ps(f"bank{i}", [128, 512], F32) for i in range(8)]
        io = {}

        def inp(name, shape, dt=F32):
            io[name] = nc.dram_tensor(name, list(shape), dt, kind="ExternalInput").ap()

        inp("x_nat", [SEQ, D])
        io["x_own"] = io["x_nat"]
        inp("c2", [128, 8, 2])
        inp("ada_w", [D, 6 * D])
        inp("ada_b", [1, 6 * D])
        inp("ada_bT", [128, 6, 8])
        inp("ident", [128, 128])
        if mode in ("full", "B"):
            inp("w_out", [D, D])
            inp("ln1_g", [1, D]); inp("ln1_b", [1, D]); inp("ln2_g", [1, D]); inp("ln2_b", [1, D])
            inp("router_w", [D, NE]); inp("router_b", [1, NE])
            inp("exp_w1", [NE, D, 2 * D]); inp("exp_w2", [NE, D, D])
            inp("b1T", [128, NE, 8, 2]); inp("exp_b2", [NE, D])
            io["out"] = nc.dram_tensor("out", [OWN, D], F32, kind="ExternalOutput").ap()
        if mode in ("full", "A1", "A2", "A12"):
            inp("ctx_nat", [256, D])
            inp("w_q", [D, 256]); inp("w_k", [D, 256]); inp("w_v", [D, 512]); inp("w_g", [D, 512])
            for d in "fb":
                inp(f"aT_{d}", [16, D]); inp(f"wa_{d}", [16, 256])
            inp("ba", [1, 512]); inp("normg", [1, 128])
            for nm in ("Uincl", "Lincl", "Ustr", "Lstr"):
                inp(nm, [128, 128])
        if mode in ("full", "A2", "A12"):
            inp("x_perm", [SEQ, D])
            for nm, shp in (("F1f", [128, 256]), ("F1d", [128, 256]), ("TWc4", [128, 1024]), ("TWs4", [128, 1024]), ("TW2c4", [128, 1024]),
                            ("TW2s4", [128, 1024]), ("C2", [128, 128]), ("S2", [128, 128]), ("nS2", [128, 128]), ("R3a", [128, 256]),
                            ("R3b", [128, 256]), ("C4", [128, 64]), ("nS4", [128, 64]), ("nC2", [128, 128]), ("nR3a", [128, 256]), ("nC4", [128, 64])):
                inp(nm, shp, BF16)
            inp("tnl", [128, 128]); inp("zT", [33, HY_N])
            inp("hy_w1", [33, 64]); inp("hy_w2", [64, 64]); inp("hy_fb", [64, 3])
            inp("w_hy_p", [4, D, 384]); inp("cw_p", [4, 3, 384]); inp("cb_p", [4, 1, 384])
            inp("wo_p", [4, 64, 3, 2, 128]); inp("db_p", [4, 1, 256])
        C = {}
        C["ident"] = S.sb("ident_sb", [128, 128], F32)
        C["ones"] = S.sb("ones", [128, 128], F32)
        C["eps"] = S.sb("epsb", [128, 1], F32)
        S.dma("sp", C["ident"][:, :], io["ident"], writes=[C["ident"]])
        S.memset("dve", C["ones"], C["ones"][:, :], 1.0)
        S.memset("dve", C["eps"], C["eps"][:, :], EPS)
        gates = S.sb("gates", [128, 32, NE], F32)
        M = build_mod(S, st, io, C)
        if DEBUG.get("stop") == "mod":
            S.emit()
            return nc
        if mode == "B":
            inp("ymT_in", [32, 128, 8, 128], BF16)
            io["ymT"] = io["ymT_in"]
            io["ymT_buf"] = Buf("ymT_in_d")
        else:
            if mode == "full":
                ymT_d = S.dram("ymT_d", [32, 128, 8, 128], BF16)
            else:
                ymT_d = Buf("ymT_o_d", nc.dram_tensor("ymT_o", [32, 128, 8, 128], BF16, kind="ExternalOutput").ap())
            io["ymT"] = ymT_d.t
            io["ymT_buf"] = ymT_d
            hT_d = S.dram("hT_d", [66, 128, 8, 128], BF16)
            hT2_d = S.dram("hT2_d", [64, 128, 8, 128], BF16) if mode in ("full", "A2", "A12") else None
            a0 = PhaseA0(S, io, C, M, hT_d, hT2_d)
            a0.first()
            a0.finish()
            if DEBUG.get("stop") != "a0" and mode in ("full", "A1", "A12"):
                phase_A1(S, io, C, hT_d, ymT_d)
            if mode in ("full", "A2", "A12"):
                phase_A2(S, io, C, hT2_d, ymT_d)
        if mode in ("full", "B"):
            phase_B(S, io, C, M, gates, NT=NT)
        S.emit()
    return nc


def _core_inputs(inp, b, th, T):
    f32 = np.float32
    x = inp["x"][b]
    ctx = inp["ctx"][b]
    if th == 1:
        x = x[::-1]
        ctx = ctx[::-1]
    x = np.ascontiguousarray(x, dtype=f32)
    m = {}
    m["x_nat"] = x
    m["x_perm"] = np.ascontiguousarray(x.reshape(64, 128, D).transpose(1, 0, 2).reshape(SEQ, D))
    m["ctx_nat"] = np.ascontiguousarray(ctx, dtype=f32)
    c2 = np.zeros((128, 8, 2), f32)
    c2[:, :, 0] = inp["c"][b].reshape(8, 128).T
    c2[:, :, 1] = inp["c_ctx"].reshape(8, 128).T
    m["c2"] = c2
    m["ada_w"] = inp["ada_w"][0]
    m["ada_b"] = inp["ada_b"][0][None, :]
    m["ada_bT"] = np.ascontiguousarray(inp["ada_b"][0].reshape(6, 8, 128).transpose(2, 0, 1))
    m["ident"] = np.eye(128, dtype=f32)
    m["w_out"] = inp["w_out"][0]
    for k in ("ln1_g", "ln1_b", "ln2_g", "ln2_b", "router_b"):
        m[k] = inp[k].reshape(1, -1)
    m["router_w"] = inp["router_w"][0]
    m["exp_w1"] = inp["exp_w1"][0]
    m["exp_w2"] = inp["exp_w2"][0]
    m["b1T"] = np.ascontiguousarray(inp["exp_b1"][0].reshape(NE, 8, 128, 2).transpose(2, 0, 1, 3))
    m["exp_b2"] = inp["exp_b2"][0]
    w_in = inp["w_in"][0]
    m["w_q"] = np.ascontiguousarray(w_in[:, 0:256])
    m["w_k"] = np.ascontiguousarray(w_in[:, 256:512])
    m["w_v"] = np.ascontiguousarray(w_in[:, 512:1024])
    m["w_g"] = np.ascontiguousarray(w_in[:, 1024:1536])
    af, ab = w_in[:, 1536:1552], w_in[:, 1552:1568]
    waf, wab = inp["gla_wa_f"][0], inp["gla_wa_b"][0]
    baf, bab = inp["gla_ba_f"][0], inp["gla_ba_b"][0]
    if th == 1:
        af, ab, waf, wab, baf, bab = ab, af, wab, waf, bab, baf
    m["aT_f"] = np.ascontiguousarray(af.T)
    m["aT_b"] = np.ascontiguousarray(ab.T)
    m["wa_f"] = np.ascontiguousarray(waf)
    m["wa_b"] = np.ascontiguousarray(wab)
    m["ba"] = np.concatenate([baf, bab])[None, :].astype(f32)
    m["normg"] = inp["gla_norm_g"].reshape(1, 128)
    i = np.arange(128)
    m["Uincl"] = (i[:, None] <= i[None, :]).astype(f32)
    m["Lincl"] = (i[:, None] >= i[None, :]).astype(f32)
    m["Ustr"] = (i[:, None] < i[None, :]).astype(f32)
    m["Lstr"] = (i[:, None] > i[None, :]).astype(f32)
    m.update(T)
    cw = inp["hy_conv_w"][0]
    if th == 1:
        cw = cw[::-1]

    def per_pass(a):
        a3 = a.reshape(a.shape[:-1] + (3, 4, 128))
        a3 = np.moveaxis(a3, -2, 0)
        return np.ascontiguousarray(a3.reshape((4,) + a.shape[:-1] + (384,)))

    m["w_hy_p"] = per_pass(w_in[:, 1568:3104])
    m["cw_p"] = per_pass(cw)
    m["cb_p"] = per_pass(inp["hy_conv_b"][0][None, :])
    wout = inp["hy_flt_wout"][0].reshape(64, 2, 2, 512)
    dA, dB = (0, 1) if th == 0 else (1, 0)
    slots = np.stack([wout[:, :, dA, :], wout[:, :, dB, :], wout[:, :, 0, :]], 1)
    m["wo_p"] = np.ascontiguousarray(slots.reshape(64, 3, 2, 4, 128).transpose(3, 0, 1, 2, 4))
    m["db_p"] = np.ascontiguousarray(inp["hy_bias_d"][0].reshape(2, 4, 128).transpose(1, 0, 2).reshape(4, 1, 256))
    m["hy_w1"] = inp["hy_flt_w1"][0]
    m["hy_w2"] = inp["hy_flt_w2"][0]
    m["hy_fb"] = np.ascontiguousarray(np.stack([inp["hy_flt_b1"][0], inp["hy_flt_b2"][0], inp["hy_flt_freq"][0]], 1))
    return m


def kernel(**inputs):
    inp = {k: np.asarray(v) for k, v in inputs.items()}
    T = hyena_tables()
    nc = build("full")
    in_maps = [_core_inputs(inp, core // 2, core % 2, T) for core in range(8)]
    res = run_bass_kernel_spmd(nc, in_maps, core_ids=list(range(8)))
    out = np.empty((4, SEQ, D), np.float32)
    for core in range(8):
        b, th = core // 2, core % 2
        o = np.asarray(res.results[core]["out"])
        if th == 0:
            out[b, :OWN] = o
        else:
            out[b, OWN:] = o[::-1]
    return out
```

```python
import numpy as np
import concourse.bass as bass
import concourse.mybir as mybir
from concourse.bass_utils import run_bass_kernel_spmd
from contextlib import ExitStack

F32 = mybir.dt.float32
BF16 = mybir.dt.bfloat16
AF = mybir.ActivationFunctionType
OP = mybir.AluOpType

D = 1024
SEQ = 8192
OWN = 4096
NE = 32
ALPHA = 2.0 ** 0.25
EPS = 1e-6
DEBUG = {}


class Buf:
    __slots__ = ("name", "t", "w", "r", "dsem", "dcnt", "dkey")

    def __init__(self, name, t=None):
        self.name = name
        self.t = t
        self.w = None
        self.r = {}
        self.dsem = None
        self.dcnt = 0
        self.dkey = None

    def __getitem__(self, idx):
        return self.t[idx]


class PB:
    __slots__ = ("S", "idx", "gen")

    def __init__(self, S, idx, gen):
        self.S = S
        self.idx = idx
        self.gen = gen

    def __getitem__(self, i):
        return self.S.banks[self.idx].t[i]


class Sched:
    def __init__(self, nc, stack):
        self.nc = nc
        self.stack = stack
        self.engs = {}
        for nm in ("pe", "act", "dve", "pool", "sp"):
            sem = stack.enter_context(nc.semaphore(f"s_{nm}"))
            self.engs[nm] = dict(key=f"E{nm}", sem=sem, cnt=0, ops=[], seen={})
        self.nd = 0
        self.dsems = []
        self.freesems = []
        self.swsems = set()
        self.bank_i = 0
        self.banks = []
        self.bankgen = {}

    def sb(self, name, shape, dt, stack=None):
        self.nsb = getattr(self, "nsb", 0) + 1
        t = (stack or self.stack).enter_context(self.nc.sbuf_tensor(f"{name}_s{self.nsb}", list(shape), dt))
        b = Buf(name, t)
        if stack is not None:
            stack.callback(self._release, b)
        return b

    def _release(self, b):
        if b.dsem is not None:
            if b.dkey not in self.swsems:
                self.freesems.append((b.dsem, b.dkey, b.dcnt))
            b.dsem = None

    def ps(self, name, shape, dt):
        t = self.stack.enter_context(self.nc.psum_tensor(name, list(shape), dt))
        return Buf(name, t)

    def dram(self, name, shape, dt, kind="Internal"):
        t = self.nc.dram_tensor(name, list(shape), dt, kind=kind)
        return Buf(name, t.ap())

    def bank(self):
        i = self.bank_i % len(self.banks)
        self.bank_i += 1
        self.bankgen[i] = self.bankgen.get(i, 0) + 1
        return PB(self, i, self.bankgen[i])

    def _norm(self, bs):
        out = []
        for x in bs:
            if isinstance(x, PB):
                assert x.gen == self.bankgen[x.idx], f"stale PSUM bank ref {x.idx}"
                out.append(self.banks[x.idx])
            else:
                out.append(x)
        return out

    def _dsem(self, b, q="sp"):
        if b.dsem is None and self.freesems and q != "pool":
            b.dsem, b.dkey, b.dcnt = self.freesems.pop()
            self.dsems.append(b)
        if b.dsem is None:
            self.nd += 1
            b.dsem = self.stack.enter_context(self.nc.semaphore(f"d{self.nd}"))
            b.dkey = f"D{self.nd}"
            self.dsems.append(b)
        return b.dsem

    def _waits(self, E, reads, writes, skipkey=None):
        waits = {}
        is_pe = E["key"] == "Epe"

        def need(rec, waw=False):
            if rec is None:
                return
            key, h, val = rec
            if key == skipkey:
                return
            if key == E["key"] and is_pe and waw:
                return
            if E["seen"].get(key, 0) >= val:
                return
            if key in waits and waits[key][1] >= val:
                return
            waits[key] = (h, val)

        for b in reads:
            need(b.w)
            if b.name.startswith("bank"):
                for rec in b.r.values():
                    if rec[0] != E["key"]:
                        need(rec)
        for b in writes:
            need(b.w, waw=True)
            for rec in b.r.values():
                need(rec)
        for key, (h, val) in waits.items():
            E["seen"][key] = val
        return list(waits.values())

    def op(self, eng, fn, reads=(), writes=()):
        E = self.engs[eng]
        reads = self._norm(reads)
        writes = self._norm(writes)
        waits = self._waits(E, reads, writes)
        E["cnt"] += 1
        rec = (E["key"], E["sem"], E["cnt"])
        E["ops"].append((waits, fn, E["sem"], 1))
        for b in reads:
            b.r[E["key"]] = rec
        for b in writes:
            b.w = rec
            b.r = {}

    def dma(self, q, out_ap, in_ap, reads=(), writes=(), owner=None, **kw):
        Q = self.engs[q]
        reads = self._norm(reads)
        writes = self._norm(writes)
        if owner is None:
            cands = [b for b in list(writes) + list(reads) if not b.name.endswith("_d")]
            owner = cands[0]
        dsem = self._dsem(owner, q)
        if q == "pool":
            self.swsems.add(owner.dkey)
        waits = self._waits(Q, reads, writes, skipkey=owner.dkey)
        owner.dcnt += 16
        rec = (owner.dkey, dsem, owner.dcnt)

        def fn(e, out_ap=out_ap, in_ap=in_ap, kw=kw):
            return e.dma_start(out=out_ap, in_=in_ap, **kw)

        Q["ops"].append((waits, fn, dsem, 16))
        for b in reads:
            b.r[owner.dkey] = rec
        for b in writes:
            b.w = rec
            b.r = {}

    def barrier(self):
        targets = []
        for nm, E in self.engs.items():
            if E["cnt"] > 0:
                targets.append((E["key"], E["sem"], E["cnt"]))
        for b in self.dsems:
            if b.dcnt > 0 and b.dsem is not None:
                targets.append((b.dkey, b.dsem, b.dcnt))
        self.dsems = [b for b in self.dsems if b.dsem is not None]
        for nm, E in self.engs.items():
            waits = []
            for key, h, val in targets:
                if key == E["key"]:
                    continue
                if E["seen"].get(key, 0) >= val:
                    continue
                E["seen"][key] = val
                waits.append((h, val))
            if waits:
                E["ops"].append((waits, None, None, 0))

    def emit(self):
        nc = self.nc
        self.barrier()
        with nc.Block() as block:
            def runner(nm):
                E = self.engs[nm]

                def body(e):
                    for waits, fn, sem, inc in E["ops"]:
                        for h, val in waits:
                            e.wait_ge(h, val)
                        if fn is not None:
                            inst = fn(e)
                            inst.then_inc(sem, inc)
                return body

            block.tensor(runner("pe"))
            block.scalar(runner("act"))
            block.vector(runner("dve"))
            block.gpsimd(runner("pool"))
            block.sync(runner("sp"))

    def mm(self, ob, out, lb, lhsT, rb, rhs, start, stop):
        self.op("pe", lambda e: e.matmul(out, lhsT=lhsT, rhs=rhs, start=start, stop=stop), reads=[lb, rb], writes=[ob])

    def tr(self, ob, out, ib, in_, idb, ident):
        self.op("pe", lambda e: e.transpose(out, in_, ident), reads=[ib, idb], writes=[ob])

    def act(self, ob, out, ib, in_, func, bias=None, scale=None, extra=(), accum=None, accb=None):
        kw = {}
        if bias is not None:
            kw["bias"] = bias
        if scale is not None:
            kw["scale"] = scale
        if accum is not None:
            kw["accum_out"] = accum
        w = [ob] + ([accb] if accb is not None else [])
        self.op("act", lambda e: e.activation(out=out, in_=in_, func=func, **kw), reads=[ib] + list(extra), writes=w)

    def ts(self, eng, ob, out, ib, in0, s1, s2, op0, op1=None, extra=()):
        if op1 is None:
            self.op(eng, lambda e: e.tensor_scalar(out=out, in0=in0, scalar1=s1, scalar2=None, op0=op0), reads=[ib] + list(extra), writes=[ob])
        else:
            self.op(eng, lambda e: e.tensor_scalar(out=out, in0=in0, scalar1=s1, scalar2=s2, op0=op0, op1=op1), reads=[ib] + list(extra), writes=[ob])

    def tt(self, eng, ob, out, ab, in0, bb, in1, op):
        self.op(eng, lambda e: e.tensor_tensor(out=out, in0=in0, in1=in1, op=op), reads=[ab, bb], writes=[ob])

    def stt(self, ob, out, ab, in0, scalar, bb, in1, op0, op1, extra=()):
        self.op("dve", lambda e: e.scalar_tensor_tensor(out=out, in0=in0, scalar=scalar, in1=in1, op0=op0, op1=op1),
                reads=[ab, bb] + list(extra), writes=[ob])

    def cp(self, eng, ob, out, ib, in_):
        if eng == "act":
            self.op("act", lambda e: e.activation(out=out, in_=in_, func=AF.Copy), reads=[ib], writes=[ob])
            return
        self.op(eng, lambda e: e.tensor_copy(out=out, in_=in_), reads=[ib], writes=[ob])

    def memset(self, eng, ob, ap, val):
        self.op(eng, lambda e: e.memset(ap, val), writes=[ob])


def ln_stats(S, xb, xap, stats, mv, rstd, epsb):
    for i in range(2):
        S.op("dve", (lambda i: lambda e: e.bn_stats(out=stats[:, i * 6:(i + 1) * 6], in_=xap[:, i * 512:(i + 1) * 512]))(i),
             reads=[xb], writes=[stats])
    S.op("dve", lambda e: e.bn_aggr(out=mv[:, 0:2], in_=stats[:, 0:12]), reads=[stats], writes=[mv])
    S.act(rstd, rstd[:, 0:1], mv, mv[:, 1:2], AF.Sqrt, bias=epsb[:, 0:1], scale=1.0, extra=[epsb])
    S.op("dve", lambda e: e.reciprocal(out=rstd[:, 0:1], in_=rstd[:, 0:1]), reads=[rstd], writes=[rstd])


def build_mod(S, st, io, C):
    nc = S.nc
    modT = S.sb("modT", [128, 6, 8], F32)
    modTc = S.sb("modTc", [128, 6, 8], F32)
    g1bc = S.sb("g1bc", [128, 1024], F32)
    g2bc = S.sb("g2bc", [128, 1024], F32)
    with ExitStack() as ls:
        cc = S.sb("cc", [128, 8, 2], F32, ls)
        sc = S.sb("sc", [128, 8, 2], F32, ls)
        scb = S.sb("scb", [128, 8, 128], F32, ls)
        adab = S.sb("adab", [128, 6, 8], F32, ls)
        adabrow = S.sb("adabrow", [1, 6144], F32, ls)
        slabs = [S.sb(f"slab{i}", [128, 8, 512], F32, ls) for i in range(2)]
        S.dma("sp", cc[:, :, :], io["c2"], writes=[cc])
        S.dma("sp", adab[:, :, :], io["ada_bT"], writes=[adab])
        S.dma("sp", adabrow[:, :], io["ada_b"], writes=[adabrow])
        S.act(sc, sc[:, :, :], cc, cc[:, :, :], AF.Silu)
        for kc in range(8):
            S.ts("dve", scb, scb[:, kc, :], C["ones"], C["ones"][:, 0:128], sc[:, kc, 0:1], None, OP.mult, extra=[sc])
        for s in range(12):
            part, half = s // 2, s % 2
            slab = slabs[s % 2]
            for kh in range(2):
                S.dma("sp", slab[:, kh * 4:(kh + 1) * 4, :],
                      io["ada_w"][kh * 512:(kh + 1) * 512, s * 512:(s + 1) * 512].rearrange("(kc k) n -> k kc n", k=128),
                      writes=[slab])
            pb = S.bank()
            for dc in range(4):
                for kc in range(8):
                    S.mm(pb, pb[:, dc * 2:dc * 2 + 2], slab, slab[:, kc, dc * 128:(dc + 1) * 128], sc, sc[:, kc, 0:2], kc == 0, kc == 7)
            for dc in range(4):
                dci = half * 4 + dc
                S.ts("dve", modT, modT[:, part, dci:dci + 1], pb, pb[:, dc * 2:dc * 2 + 1], adab[:, part, dci:dci + 1], None, OP.add, extra=[adab])
                S.ts("dve", modTc, modTc[:, part, dci:dci + 1], pb, pb[:, dc * 2 + 1:dc * 2 + 2], adab[:, part, dci:dci + 1], None, OP.add, extra=[adab])
            if part in (2, 5):
                pb2 = S.bank()
                for kc in range(8):
                    S.mm(pb2, pb2[:, :], scb, scb[:, kc, :], slab, slab[:, kc, :], kc == 0, False)
                S.mm(pb2, pb2[:, :], C["ones"], C["ones"][0:1, 0:128], adabrow, adabrow[0:1, s * 512:(s + 1) * 512], False, True)
                dst = g1bc if part == 2 else g2bc
                S.cp("dve", dst, dst[:, half * 512:(half + 1) * 512], pb2, pb2[:, :])
        S.barrier()
    return dict(modT=modT, modTc=modTc, g1bc=g1bc, g2bc=g2bc)


def bcast_row(S, C, dst, row_ap, rowbuf, n):
    S.dma("sp", rowbuf[0:1, 0:n], row_ap, writes=[rowbuf])
    for h in range(0, n, 512):
        w = min(512, n - h)
        pb = S.bank()
        S.mm(pb, pb[:, 0:w], C["ones"], C["ones"][0:1, 0:128], rowbuf, rowbuf[0:1, h:h + w], True, True)
        S.cp("dve", dst, dst[:, h:h + w], pb, pb[:, 0:w])


def phase_B(S, io, C, M, gates, NT=32):
    nc = S.nc
    modT, g1bc, g2bc = M["modT"], M["g1bc"], M["g2bc"]
    xmid_d = S.dram("xmid_d", [NT, 128, 1024], F32)
    h2T_d = S.dram("h2T_d", [NT, 128, 8, 128], BF16)
    with ExitStack() as ls:
        wo = S.sb("wo", [128, 8, 1024], BF16, ls)
        rw = S.sb("rw", [128, 8, 32], F32, ls)
        rbrow = S.sb("rbrow", [1, 32], F32, ls)
        rowbuf = S.sb("rowbuf", [1, 1024], F32, ls)
        ln1g = S.sb("ln1g", [128, 1024], F32, ls)
        ln1b = S.sb("ln1b", [128, 1024], F32, ls)
        sc4 = S.sb("sc4", [128, 8], F32, ls)
        xt = [S.sb(f"xt{i}", [128, 1024], F32, ls) for i in range(3)]
        ym = [S.sb(f"ym{i}", [128, 8, 128], BF16, ls) for i in range(3)]
        r = [S.sb(f"r{i}", [128, 1024], F32, ls) for i in range(3)]
        xm = [S.sb(f"xm{i}", [128, 1024], F32, ls) for i in range(3)]
        xn2 = [S.sb(f"xn2{i}", [128, 1024], F32, ls) for i in range(3)]
        h2b = [S.sb(f"h2b{i}", [128, 8, 128], BF16, ls) for i in range(3)]
        h2f = [S.sb(f"h2f{i}", [128, 8, 128], F32, ls) for i in range(3)]
        def ring2(nm, shp):
            return [S.sb(f"{nm}{i}", shp, F32, ls) for i in range(3)]
        statsA, mvA, rstdA = ring2("statsA", [128, 12]), ring2("mvA", [128, 2]), ring2("rstdA", [128, 1])
        statsB, mvB, rstdB = ring2("statsB", [128, 12]), ring2("mvB", [128, 2]), ring2("rstdB", [128, 1])
        lg_, t8_, nv1_, msk_, ex_, ssum_ = (ring2("lg", [128, 32]), ring2("t8", [128, 8]), ring2("nv1", [128, 1]),
                                            ring2("msk", [128, 32]), ring2("ex", [128, 32]), ring2("ssum", [128, 1]))
        S.dma("pool", wo[:, :, :], io["w_out"].rearrange("(kc k) n -> k kc n", k=128), writes=[wo])
        S.dma("sp", rw[:, :, :], io["router_w"].rearrange("(kc k) n -> k kc n", k=128), writes=[rw])
        S.dma("sp", rbrow[:, :], io["router_b"], writes=[rbrow])
        bcast_row(S, C, ln1g, io["ln1_g"], rowbuf, 1024)
        bcast_row(S, C, ln1b, io["ln1_b"], rowbuf, 1024)
        S.ts("dve", sc4, sc4[:, :], modT, modT[:, 4, :], 1.0, None, OP.add)
        def front_tile(tt):
            i = tt % 3
            sA, mA, rA = statsA[i], mvA[i], rstdA[i]
            sB, mB, rB = statsB[i], mvB[i], rstdB[i]
            st_ = {}

            def f1():
                S.dma("sp", xt[i][:, :], io["x_own"][tt * 128:(tt + 1) * 128, :], writes=[xt[i]])
                S.dma("sp", ym[i][:, :, :], io["ymT"][tt], writes=[ym[i]], reads=[io["ymT_buf"]])
                pf = [S.bank(), S.bank()]
                for h in range(2):
                    for kc in range(8):
                        S.mm(pf[h], pf[h][:, :], ym[i], ym[i][:, kc, :], wo, wo[:, kc, h * 512:(h + 1) * 512], kc == 0, kc == 7)
                for h in range(2):
                    S.tt("dve", r[i], r[i][:, h * 512:(h + 1) * 512], pf[h], pf[h][:, :], g1bc, g1bc[:, h * 512:(h + 1) * 512], OP.mult)
                S.stt(r[i], r[i][:, :], xt[i], xt[i][:, :], ALPHA, r[i], r[i][:, :], OP.mult, OP.add)
                for i2 in range(2):
                    S.op("dve", (lambda i2: lambda e: e.bn_stats(out=sA[:, i2 * 6:(i2 + 1) * 6], in_=r[i][:, i2 * 512:(i2 + 1) * 512]))(i2),
                         reads=[r[i]], writes=[sA])
                S.op("dve", lambda e: e.bn_aggr(out=mA[:, 0:2], in_=sA[:, 0:12]), reads=[sA], writes=[mA])
                S.act(rA, rA[:, 0:1], mA, mA[:, 1:2], AF.Sqrt, bias=C["eps"][:, 0:1], scale=1.0, extra=[C["eps"]])

            def f2():
                S.op("dve", lambda e: e.reciprocal(out=rA[:, 0:1], in_=rA[:, 0:1]), reads=[rA], writes=[rA])
                S.ts("dve", xm[i], xm[i][:, :], r[i], r[i][:, :], mA[:, 0:1], rA[:, 0:1], OP.subtract, OP.mult, extra=[mA, rA])
                S.tt("pool", xm[i], xm[i][:, :], xm[i], xm[i][:, :], ln1g, ln1g[:, :], OP.mult)
                S.tt("pool", xm[i], xm[i][:, :], xm[i], xm[i][:, :], ln1b, ln1b[:, :], OP.add)
                S.dma("pool", xmid_d[tt], xm[i][:, :], reads=[xm[i]], writes=[xmid_d])
                for i2 in range(2):
                    S.op("dve", (lambda i2: lambda e: e.bn_stats(out=sB[:, i2 * 6:(i2 + 1) * 6], in_=xm[i][:, i2 * 512:(i2 + 1) * 512]))(i2),
                         reads=[xm[i]], writes=[sB])
                S.op("dve", lambda e: e.bn_aggr(out=mB[:, 0:2], in_=sB[:, 0:12]), reads=[sB], writes=[mB])
                S.act(rB, rB[:, 0:1], mB, mB[:, 1:2], AF.Sqrt, bias=C["eps"][:, 0:1], scale=1.0, extra=[C["eps"]])

            def f3():
                S.op("dve", lambda e: e.reciprocal(out=rB[:, 0:1], in_=rB[:, 0:1]), reads=[rB], writes=[rB])
                S.ts("dve", xn2[i], xn2[i][:, :], xm[i], xm[i][:, :], mB[:, 0:1], rB[:, 0:1], OP.subtract, OP.mult, extra=[mB, rB])
                st_["pt"] = [S.bank(), S.bank()]
                for kc in range(8):
                    p = st_["pt"][kc // 4]
                    S.tr(p, p[:, (kc % 4) * 128:(kc % 4 + 1) * 128], xn2[i], xn2[i][:, kc * 128:(kc + 1) * 128], C["ident"], C["ident"][:, :])

            def f4():
                for kc in range(8):
                    p = st_["pt"][kc // 4]
                    src_ = p[:, (kc % 4) * 128:(kc % 4 + 1) * 128]
                    S.ts("dve", h2f[i], h2f[i][:, kc, :], p, src_, sc4[:, kc:kc + 1], modT[:, 3, kc:kc + 1], OP.mult, OP.add, extra=[modT, sc4])
                S.cp("act", h2b[i], h2b[i][:, :, :], h2f[i], h2f[i][:, :, :])
                S.dma("act", h2T_d[tt], h2b[i][:, :, :], reads=[h2b[i]], writes=[h2T_d])
                pl = S.bank()
                for kc in range(8):
                    S.mm(pl, pl[:, 0:32], h2f[i], h2f[i][:, kc, :], rw, rw[:, kc, :], kc == 0, False)
                S.mm(pl, pl[:, 0:32], C["ones"], C["ones"][0:1, 0:128], rbrow, rbrow[0:1, :], False, True)
                st_["pl"] = pl

            def f5():
                pl = st_["pl"]
                lg, t8, nv1, msk, ex, ssum = lg_[i], t8_[i], nv1_[i], msk_[i], ex_[i], ssum_[i]
                S.cp("dve", lg, lg[:, :], pl, pl[:, 0:32])
                S.op("dve", lambda e: e.max(out=t8[:, :], in_=lg[:, :]), reads=[lg], writes=[t8])
                S.ts("dve", nv1, nv1[:, :], t8, t8[:, 0:1], -1.0, None, OP.mult)
                S.ts("dve", msk, msk[:, :], lg, lg[:, :], t8[:, 3:4], None, OP.is_ge, extra=[t8])
                S.act(ex, ex[:, :], lg, lg[:, :], AF.Exp, bias=nv1[:, 0:1], scale=1.0, extra=[nv1])
                S.tt("dve", ex, ex[:, :], ex, ex[:, :], msk, msk[:, :], OP.mult)
                S.op("dve", lambda e: e.reduce_sum(out=ssum[:, :], in_=ex[:, :], axis=mybir.AxisListType.X), reads=[ex], writes=[ssum])
                S.op("dve", lambda e: e.reciprocal(out=ssum[:, :], in_=ssum[:, :]), reads=[ssum], writes=[ssum])
                S.ts("dve", gates, gates[:, tt, :], ex, ex[:, :], ssum[:, 0:1], None, OP.mult, extra=[ssum])

            return [f1, f2, f3, f4, f5]

        funits = [front_tile(tt) for tt in range(NT)]
        for slot in range(NT + 4):
            for s_ in (4, 3, 2, 1, 0):
                t_ = slot - s_
                if 0 <= t_ < NT:
                    funits[t_][s_]()
        S.barrier()
    if DEBUG.get("skip_moe"):
        return
    with ExitStack() as ls:
        QT = 8
        acc = S.sb("acc", [128, QT, 1024], F32, ls)
        h2T = S.sb("h2T", [128, 8, QT * 128], BF16, ls)
        w1 = [S.sb(f"w1_{i}", [128, 8, 2048], BF16, ls) for i in range(2)]
        w2 = [S.sb(f"w2_{i}", [128, 8, 1024], BF16, ls) for i in range(2)]
        actT = [S.sb(f"actT{i}", [128, 8, 512], BF16, ls) for i in range(2)]
        gsb = [S.sb(f"g{i}", [128, 512], F32, ls) for i in range(2)]
        gss = [S.sb(f"gs{i}", [128, 512], F32, ls) for i in range(2)]
        asb = [S.sb(f"a{i}", [128, 512], F32, ls) for i in range(2)]
        b1 = S.sb("b1", [128, NE, 8, 2], F32, ls)
        b2s = S.sb("b2s", [32, 1024], F32, ls)
        gT = S.sb("gT", [32, 128], F32, ls)
        ln2g = S.sb("ln2g", [128, 1024], F32, ls)
        ln2b = S.sb("ln2b", [128, 1024], F32, ls)
        stats = S.sb("stats2", [128, 12], F32, ls)
        mv = S.sb("mv2", [128, 2], F32, ls)
        rstd = S.sb("rstd2", [128, 1], F32, ls)
        with ExitStack() as l2:
            rowbuf = S.sb("rowbuf2", [1, 1024], F32, l2)
            bcast_row(S, C, ln2g, io["ln2_g"], rowbuf, 1024)
            bcast_row(S, C, ln2b, io["ln2_b"], rowbuf, 1024)
            S.barrier()
        S.dma("sp", b1[:, :, :, :], io["b1T"], writes=[b1])
        S.ts("dve", b1, b1[:, :, :, 1:2], b1, b1[:, :, :, 1:2], 1.0, None, OP.add)
        S.dma("sp", b2s[:, :], io["exp_b2"], writes=[b2s])
        S.tt("dve", b2s, b2s[:, :], b2s, b2s[:, :], g2bc, g2bc[0:32, :], OP.mult)
        ei = 0
        for qt in range(NT // QT):
            for t in range(QT):
                tt = qt * QT + t
                S.dma("sp", h2T[:, :, t * 128:(t + 1) * 128], h2T_d[tt], writes=[h2T], reads=[h2T_d])
                S.dma("sp", acc[:, t, :], xmid_d[tt], writes=[acc], reads=[xmid_d])
            S.ts("dve", acc, acc[:, :, :], acc, acc[:, :, :], ALPHA, None, OP.mult)
            for e in range(NE):
                wb1 = w1[ei % 2]
                wb2 = w2[ei % 2]
                ei += 1
                for kh in range(4):
                    S.dma("pool", wb1[:, kh * 2:(kh + 1) * 2, :],
                          io["exp_w1"][e, kh * 256:(kh + 1) * 256, :].rearrange("(kc k) n -> k kc n", k=128), writes=[wb1])
                for kh in range(2):
                    S.dma("pool", wb2[:, kh * 4:(kh + 1) * 4, :],
                          io["exp_w2"][e, kh * 512:(kh + 1) * 512, :].rearrange("(kc k) n -> k kc n", k=128), writes=[wb2])
                for kc in range(8):
                    S.tt("pool", wb2, wb2[:, kc, :], wb2, wb2[:, kc, :], g2bc, g2bc[:, :], OP.mult)
                for tb in range(QT // 4):
                    at = actT[tb % 2]
                    for hc in range(8):
                        pg, pl_ = S.bank(), S.bank()
                        for kc in range(8):
                            S.mm(pg, pg[:, :], wb1, wb1[:, kc, hc * 256:(hc + 1) * 256:2], h2T, h2T[:, kc, tb * 512:(tb + 1) * 512], kc == 0, kc == 7)
                        for kc in range(8):
                            S.mm(pl_, pl_[:, :], wb1, wb1[:, kc, hc * 256 + 1:(hc + 1) * 256:2], h2T, h2T[:, kc, tb * 512:(tb + 1) * 512], kc == 0, kc == 7)
                        j = hc % 2
                        S.ts("dve", gsb[j], gsb[j][:, :], pg, pg[:, :], b1[:, e, hc, 0:1], 7.0, OP.add, OP.min, extra=[b1])
                        S.act(gss[j], gss[j][:, :], gsb[j], gsb[j][:, :], AF.Gelu_apprx_sigmoid)
                        S.ts("dve", asb[j], asb[j][:, :], pl_, pl_[:, :], b1[:, e, hc, 1:2], -6.0, OP.add, OP.max, extra=[b1])
                        S.stt(at, at[:, hc, :], asb[j], asb[j][:, :], 8.0, gss[j], gss[j][:, :], OP.min, OP.mult)
                    for t4 in range(4):
                        t = tb * 4 + t4
                        tt = qt * QT + t
                        for dh in range(2):
                            py = S.bank()
                            for hc in range(8):
                                S.mm(py, py[:, :], at, at[:, hc, t4 * 128:(t4 + 1) * 128], wb2, wb2[:, hc, dh * 512:(dh + 1) * 512], hc == 0, hc == 7)
                            S.stt(acc, acc[:, t, dh * 512:(dh + 1) * 512], py, py[:, :], gates[:, tt, e:e + 1], acc, acc[:, t, dh * 512:(dh + 1) * 512],
                                  OP.mult, OP.add, extra=[gates])
            for t in range(QT):
                tt = qt * QT + t
                pgt = S.bank()
                S.tr(pgt, pgt[0:32, 0:128], gates, gates[:, tt, :], C["ident"], C["ident"][:, :])
                S.cp("dve", gT, gT[:, :], pgt, pgt[0:32, 0:128])
                for dh in range(2):
                    pb = S.bank()
                    S.mm(pb, pb[:, :], gT, gT[:, :], b2s, b2s[:, dh * 512:(dh + 1) * 512], True, True)
                    S.tt("dve", acc, acc[:, t, dh * 512:(dh + 1) * 512], pb, pb[:, :], acc, acc[:, t, dh * 512:(dh + 1) * 512], OP.add)
                a_ap = acc[:, t, :]
                for i2 in range(2):
                    S.op("dve", (lambda i2, a_ap: lambda e: e.bn_stats(out=stats[:, i2 * 6:(i2 + 1) * 6], in_=a_ap[:, i2 * 512:(i2 + 1) * 512]))(i2, a_ap),
                         reads=[acc], writes=[stats])
                S.op("dve", lambda e: e.bn_aggr(out=mv[:, 0:2], in_=stats[:, 0:12]), reads=[stats], writes=[mv])
                S.act(rstd, rstd[:, 0:1], mv, mv[:, 1:2], AF.Sqrt, bias=C["eps"][:, 0:1], scale=1.0, extra=[C["eps"]])
                S.op("dve", lambda e: e.reciprocal(out=rstd[:, 0:1], in_=rstd[:, 0:1]), reads=[rstd], writes=[rstd])
                S.ts("dve", acc, a_ap, acc, a_ap, mv[:, 0:1], rstd[:, 0:1], OP.subtract, OP.mult, extra=[mv, rstd])
                S.tt("pool", acc, a_ap, acc, a_ap, ln2g, ln2g[:, :], OP.mult)
                S.tt("pool", acc, a_ap, acc, a_ap, ln2b, ln2b[:, :], OP.add)
                S.dma("sp", io["out"][tt * 128:(tt + 1) * 128, :], a_ap, reads=[acc])
        S.barrier()


def ln_mod_stages(S, C, x_ap, xt, xt2, xn, hb, stats, mv, rstd, shiftT, sc1T, dst_ap, dst_buf):
    st_ = {}

    def a1():
        S.dma("sp", xt[0:64, :], x_ap[0:64, :], writes=[xt])
        S.dma("pool", xt[64:128, :], x_ap[64:128, :], writes=[xt2])
        for i in range(2):
            S.op("dve", (lambda i: lambda e: e.bn_stats(out=stats[:, i * 6:(i + 1) * 6], in_=xt[:, i * 512:(i + 1) * 512]))(i),
                 reads=[xt, xt2], writes=[stats])
        S.op("dve", lambda e: e.bn_aggr(out=mv[:, 0:2], in_=stats[:, 0:12]), reads=[stats], writes=[mv])
        S.act(rstd, rstd[:, 0:1], mv, mv[:, 1:2], AF.Sqrt, bias=C["eps"][:, 0:1], scale=1.0, extra=[C["eps"]])

    def a2():
        S.op("dve", lambda e: e.reciprocal(out=rstd[:, 0:1], in_=rstd[:, 0:1]), reads=[rstd], writes=[rstd])
        S.ts("dve", xn, xn[:, :], xt, xt[:, :], mv[:, 0:1], rstd[:, 0:1], OP.subtract, OP.mult, extra=[mv, rstd, xt2])
        st_["pt"] = [S.bank(), S.bank()]
        for kc in range(8):
            p = st_["pt"][kc // 4]
            S.tr(p, p[:, (kc % 4) * 128:(kc % 4 + 1) * 128], xn, xn[:, kc * 128:(kc + 1) * 128], C["ident"], C["ident"][:, :])

    def b_():
        for kc in range(8):
            p = st_["pt"][kc // 4]
            src_ = p[:, (kc % 4) * 128:(kc % 4 + 1) * 128]
            if kc < 4:
                S.act(hb, hb[:, kc, :], p, src_, AF.Identity, bias=shiftT[:, kc:kc + 1], scale=sc1T[:, kc:kc + 1], extra=[C["modsc"]])
            else:
                S.ts("dve", hb, hb[:, kc, :], p, src_, sc1T[:, kc:kc + 1], shiftT[:, kc:kc + 1], OP.mult, OP.add, extra=[C["modsc"]])
        S.dma("act", dst_ap, hb[:, :, :], reads=[hb], writes=[dst_buf])

    return [a1, a2, b_]


class PhaseA0:
    def __init__(self, S, io, C, M, hT_d, hT2_d):
        self.S = S
        modT, modTc = M["modT"], M["modTc"]
        self.ls = ls = ExitStack()
        xt = [S.sb(f"a0xt{i}", [128, 1024], F32, ls) for i in range(4)]
        xt2 = [Buf(f"a0xtH{i}", xt[i].t) for i in range(4)]
        for b_ in xt2:
            ls.callback(S._release, b_)
        xn = [S.sb(f"a0xn{i}", [128, 1024], F32, ls) for i in range(3)]
        hb = [S.sb(f"a0hb{i}", [128, 8, 128], BF16, ls) for i in range(3)]
        stats = [S.sb(f"a0stats{i}", [128, 12], F32, ls) for i in range(3)]
        mv = [S.sb(f"a0mv{i}", [128, 2], F32, ls) for i in range(3)]
        rstd = [S.sb(f"a0rstd{i}", [128, 1], F32, ls) for i in range(3)]
        msc = S.sb("a0msc", [128, 4, 8], F32, ls)
        C["modsc"] = msc
        S.cp("dve", msc, msc[:, 0, :], modT, modT[:, 0, :])
        S.ts("dve", msc, msc[:, 1, :], modT, modT[:, 1, :], 1.0, None, OP.add)
        S.cp("dve", msc, msc[:, 2, :], modTc, modTc[:, 0, :])
        S.ts("dve", msc, msc[:, 3, :], modTc, modTc[:, 1, :], 1.0, None, OP.add)
        jobs = [(io["x_nat"][t * 128:(t + 1) * 128, :], 0, hT_d[t], hT_d) for t in range(64)]
        jobs += [(io["ctx_nat"][t * 128:(t + 1) * 128, :], 2, hT_d[64 + t], hT_d) for t in range(2)]
        self.n_first = len(jobs)
        if hT2_d is not None:
            jobs += [(io["x_perm"][t * 128:(t + 1) * 128, :], 0, hT2_d[t], hT2_d) for t in range(64)]
        self.units = []
        for n, (xap, mi, dap, dbuf) in enumerate(jobs):
            self.units.append(ln_mod_stages(S, C, xap, xt[n % 4], xt2[n % 4], xn[n % 3], hb[n % 3], stats[n % 3], mv[n % 3], rstd[n % 3],
                                            msc[:, mi, :], msc[:, mi + 1, :], dap, dbuf))
        self.slot = 0

    def _emit_slot(self, lo, hi):
        for s_ in (2, 1, 0):
            t_ = self.slot - s_
            if lo <= t_ < hi:
                self.units[t_][s_]()
        self.slot += 1

    def first(self):
        while self.slot < self.n_first + 2:
            self._emit_slot(0, self.n_first)
        self.S.barrier()
        self.slot = self.n_first

    def filler(self, n=1):
        for _ in range(n):
            if self.slot < len(self.units) + 2:
                self._emit_slot(self.n_first, len(self.units))

    def finish(self):
        while self.slot < len(self.units) + 2:
            self._emit_slot(self.n_first, len(self.units))
        self.S.barrier()
        self.ls.close()


def phase_A1(S, io, C, hT_d, ymT_d, filler=lambda: None):
    with ExitStack() as ls:
        Wtok = S.sb("Wtok", [128, 8, 1792], BF16, ls)
        Wfm = S.sb("Wfm", [128, 8, 512], BF16, ls)
        barow = S.sb("barow", [1, 512], F32, ls)
        normg4 = S.sb("normg4", [128, 512], F32, ls)
        Uincl = S.sb("Uincl_sb", [128, 128], F32, ls)
        Lincl = S.sb("Lincl_sb", [128, 128], F32, ls)
        Ustr = S.sb("Ustr_sb", [128, 128], F32, ls)
        Lstr = S.sb("Lstr_sb", [128, 128], F32, ls)
        Mf4 = S.sb("Mf4", [128, 512], F32, ls)
        Mb4 = S.sb("Mb4", [128, 512], F32, ls)
        Sst = {d: S.sb(f"S_{d}", [128, 2, 128], F32, ls) for d in "fb"}
        Sfbf = S.sb("Sfbf", [128, 2, 2, 128], BF16, ls)
        Sbbf = S.sb("Sbbf", [128, 32, 2, 2, 128], BF16, ls)
        hm = S.sb("hm", [128, 2], F32, ls)
        S.memset("dve", hm, hm[0:64, 0:1], 1.0)
        S.memset("dve", hm, hm[64:128, 0:1], 0.0)
        S.memset("dve", hm, hm[0:64, 1:2], 0.0)
        S.memset("dve", hm, hm[64:128, 1:2], 1.0)
        hT = [S.sb(f"a1hT{i}", [128, 8, 128], BF16, ls) for i in range(3)]
        class R2:
            def __init__(self, mk):
                self.bs = [mk(0), mk(1)]
                self.i = 0
            @property
            def c(self):
                return self.bs[self.i % 2]
            def adv(self):
                self.i += 1
        Gs_ = {d: R2(lambda i, d=d: S.sb(f"G_{d}{i}", [128, 256], F32, ls)) for d in "fb"}
        e1_ = R2(lambda i: S.sb(f"e1{i}", [128, 256], F32, ls))
        Ekh_ = R2(lambda i: S.sb(f"Ekh{i}", [128, 256], F32, ls))
        khat_ = R2(lambda i: S.sb(f"khat{i}", [128, 256], BF16, ls))
        vbf_ = R2(lambda i: S.sb(f"vbf{i}", [128, 512], BF16, ls))
        etot_ = R2(lambda i: S.sb(f"etot{i}", [128, 4], F32, ls))
        Eq_ = R2(lambda i: S.sb(f"Eq{i}", [128, 512], F32, ls))
        Ek_ = R2(lambda i: S.sb(f"Ek{i}", [128, 512], F32, ls))
        qtl_ = R2(lambda i: S.sb(f"qtl{i}", [128, 512], BF16, ls))
        ktl_ = R2(lambda i: S.sb(f"ktl{i}", [128, 2, 512], BF16, ls))
        Af_ = R2(lambda i: S.sb(f"Af{i}", [128, 512], BF16, ls))
        Ab_ = R2(lambda i: S.sb(f"Ab{i}", [128, 512], BF16, ls))
        ssq_ = R2(lambda i: S.sb(f"ssq{i}", [128, 4], F32, ls))
        junk = S.sb("junk", [128, 128], F32, ls)
        on_ = R2(lambda i: S.sb(f"on{i}", [128, 512], F32, ls))
        sg_ = R2(lambda i: S.sb(f"sg{i}", [128, 512], F32, ls))
        rings = list(Gs_.values()) + [e1_, Ekh_, khat_, vbf_, etot_, Eq_, Ek_, qtl_, ktl_, Af_, Ab_, ssq_, on_, sg_]

        def adv_all():
            for r_ in rings:
                r_.adv()
        ymt = [S.sb(f"ymt{i}", [128, 512], BF16, ls) for i in range(2)]
        one1 = C["ones"]
        with ExitStack() as l2:
            aT = {d: S.sb(f"aT_{d}", [16, 1024], F32, l2) for d in "fb"}
            wa = {d: S.sb(f"wa_{d}", [16, 256], F32, l2) for d in "fb"}
            rowb = S.sb("a1rowb", [1, 512], F32, l2)
            for nm, dst, c0, wdt in (("w_k", Wtok, 0, 256), ("w_v", Wtok, 256, 512), ("w_g", Wtok, 768, 512)):
                S.dma("pool", dst[:, :, c0:c0 + wdt], io[nm].rearrange("(kc k) n -> k kc n", k=128), writes=[dst])
            S.dma("pool", Wfm[:, :, 0:256], io["w_q"].rearrange("(kc k) n -> k kc n", k=128), writes=[Wfm])
            S.dma("pool", Wfm[:, :, 256:512], io["w_k"].rearrange("(kc k) n -> k kc n", k=128), writes=[Wfm])
            S.ts("dve", Wfm, Wfm[:, :, 0:256], Wfm, Wfm[:, :, 0:256], 0.125, None, OP.mult)
            for d in "fb":
                S.dma("sp", aT[d][:, :], io[f"aT_{d}"], writes=[aT[d]])
                S.dma("sp", wa[d][:, :], io[f"wa_{d}"], writes=[wa[d]])
            for di, d in enumerate("fb"):
                for kc in range(8):
                    pb = S.bank()
                    S.mm(pb, pb[:, 0:256], aT[d], aT[d][:, kc * 128:(kc + 1) * 128], wa[d], wa[d][:, :], True, True)
                    S.cp("dve", Wtok, Wtok[:, kc, 1280 + di * 256:1536 + di * 256], pb, pb[:, 0:256])
            S.dma("sp", barow[:, :], io["ba"], writes=[barow])
            for nm, dst in (("Uincl", Uincl), ("Lincl", Lincl), ("Ustr", Ustr), ("Lstr", Lstr)):
                S.dma("sp", dst[:, :], io[nm], writes=[dst])
            for h in range(4):
                S.cp("dve", Mf4, Mf4[:, h * 128:(h + 1) * 128], Uincl, Uincl[:, :])
                S.cp("dve", Mb4, Mb4[:, h * 128:(h + 1) * 128], Lincl, Lincl[:, :])
            S.dma("sp", rowb[0:1, 0:128], io["normg"], writes=[rowb])
            pb = S.bank()
            S.mm(pb, pb[:, 0:128], C["ones"], C["ones"][0:1, 0:128], rowb, rowb[0:1, 0:128], True, True)
            for h in range(4):
                S.cp("dve", normg4, normg4[:, h * 128:(h + 1) * 128], pb, pb[:, 0:128])
            S.barrier()
        for d in "fb":
            S.memset("dve", Sst[d], Sst[d][:, :, :], 0.0)
        hti = [0]
        if DEBUG.get("stop") == "a1prep":
            S.barrier()
            return

        def load_hT(t):
            b = hT[hti[0] % 3]
            hti[0] += 1
            S.dma("sp", b[:, :, :], hT_d[t], writes=[b], reads=[hT_d])
            return b

        def proj_tok(hb, c0, wdt, bias_c0=None):
            pb = S.bank()
            for kc in range(8):
                S.mm(pb, pb[:, 0:wdt], hb, hb[:, kc, :], Wtok, Wtok[:, kc, c0:c0 + wdt], kc == 0, (kc == 7) and bias_c0 is None)
            if bias_c0 is not None:
                S.mm(pb, pb[:, 0:wdt], C["ones"], C["ones"][0:1, 0:128], barow, barow[0:1, bias_c0:bias_c0 + wdt], False, True)
            return pb

        def make_G(d, pz):
            S.act(e1_.c, e1_.c[:, :], pz, pz[:, 0:256], AF.Exp, scale=-1.0)
            S.act(Gs_[d].c, Gs_[d].c[:, :], e1_.c, e1_.c[:, :], AF.Ln, bias=one1[:, 0:1], scale=1.0, extra=[one1])

        def su_prep(d, pk):
            strict = Lstr if d == "f" else Ustr
            pD = S.bank()
            S.mm(pD, pD[:, 0:256], strict, strict[:, :], Gs_[d].c, Gs_[d].c[:, :], True, True)
            S.act(Ekh_.c, Ekh_.c[:, :], pD, pD[:, 0:256], AF.Exp, scale=-1.0 / 16)
            S.tt("dve", khat_.c, khat_.c[:, :], pk, pk[:, 0:256], Ekh_.c, Ekh_.c[:, :], OP.mult)
            pT = S.bank()
            for p in range(2):
                S.mm(pT, pT[:, p * 2:p * 2 + 2], Gs_[d].c, Gs_[d].c[:, p * 128:(p + 1) * 128], C["ones"], C["ones"][:, 0:2], True, True)
            S.act(etot_.c, etot_.c[:, :], pT, pT[:, 0:4], AF.Exp, scale=-1.0 / 16)

        def su_apply(d, khat=None, vbf=None, etot=None):
            khat = khat or khat_.c
            vbf = vbf or vbf_.c
            etot = etot or etot_.c
            pu = S.bank()
            for h in range(4):
                p, s = h // 2, h % 2
                S.mm(pu, pu[s * 64:(s + 1) * 64, p * 128:(p + 1) * 128], khat, khat[:, h * 64:(h + 1) * 64], vbf, vbf[:, h * 128:(h + 1) * 128], True, True)
            for p in range(2):
                S.stt(Sst[d], Sst[d][:, p, :], Sst[d], Sst[d][:, p, :], etot[:, p * 2:p * 2 + 1], pu, pu[:, p * 128:(p + 1) * 128], OP.mult, OP.add, extra=[etot])

        def state_only(d, t):
            adv_all()
            filler()
            hb = load_hT(t)
            pk = proj_tok(hb, 0, 256)
            pv = proj_tok(hb, 256, 512)
            pz = proj_tok(hb, 1280 if d == "f" else 1536, 256, bias_c0=0 if d == "f" else 256)
            S.cp("act", vbf_.c, vbf_.c[:, :], pv, pv[:, :])
            make_G(d, pz)
            su_prep(d, pk)
            su_apply(d)

        state_only("f", 64); state_only("f", 65)
        state_only("b", 65); state_only("b", 64)
        if DEBUG.get("stop") == "a1ctx":
            S.barrier()
            return
        for n in range(63, -1, -1):
            if n < 32:
                for s in range(2):
                    S.ts("dve", Sbbf, Sbbf[:, n, :, s, :], Sst["b"], Sst["b"][:, :, :], hm[:, s:s + 1], None, OP.mult, extra=[hm])
            if n > 0:
                state_only("b", n)
        if DEBUG.get("stop") == "a1bwd":
            S.barrier()
            return
        def fwd_chunk(n):
            adv_all()
            hb = load_hT(n)
            vbf, sg, Eq, Ek, qtl, ktl, Af, Ab, ssq, on = vbf_.c, sg_.c, Eq_.c, Ek_.c, qtl_.c, ktl_.c, Af_.c, Ab_.c, ssq_.c, on_.c
            khat, etot = khat_.c, etot_.c
            pq = S.bank()
            for j in range(4):
                for kc in range(8):
                    S.mm(pq, pq[:, j * 128:(j + 1) * 128], Wfm, Wfm[:, kc, j * 128:(j + 1) * 128], hb, hb[:, kc, :], kc == 0, kc == 7)
            pk = proj_tok(hb, 0, 256)
            pv = proj_tok(hb, 256, 512)
            pg = proj_tok(hb, 768, 512)
            pzf = proj_tok(hb, 1280, 256, bias_c0=0)
            pzb = proj_tok(hb, 1536, 256, bias_c0=256)
            S.cp("act", vbf, vbf[:, :], pv, pv[:, :])
            S.act(sg, sg[:, :], pg, pg[:, :], AF.Silu)
            make_G("f", pzf)
            make_G("b", pzb)
            pc = S.bank()
            for di, d in enumerate("fb"):
                tri = Uincl if d == "f" else Lincl
                for p in range(2):
                    j = di * 2 + p
                    S.mm(pc, pc[:, j * 128:(j + 1) * 128], Gs_[d].c, Gs_[d].c[:, p * 128:(p + 1) * 128], tri, tri[:, :], True, True)
            S.act(Eq, Eq[:, :], pc, pc[:, :], AF.Exp, scale=-1.0 / 16)
            S.act(Ek, Ek[:, :], pc, pc[:, :], AF.Exp, scale=1.0 / 16)
            for di in range(2):
                S.tt("dve", qtl, qtl[:, di * 256:(di + 1) * 256], pq, pq[:, 0:256], Eq, Eq[:, di * 256:(di + 1) * 256], OP.mult)
                for s in range(2):
                    S.stt(ktl, ktl[:, s, di * 256:(di + 1) * 256], pq, pq[:, 256:512], hm[:, s:s + 1], Ek, Ek[:, di * 256:(di + 1) * 256],
                          OP.mult, OP.mult, extra=[hm])
            su_prep("f", pk)
            for di, (A, Mk) in enumerate(((Af, Mf4), (Ab, Mb4))):
                psc = S.bank()
                for h in range(4):
                    p, s = h // 2, h % 2
                    j = di * 2 + p
                    S.mm(psc, psc[:, h * 128:(h + 1) * 128], ktl, ktl[:, s, j * 128:(j + 1) * 128],
                         qtl, qtl[:, j * 128:(j + 1) * 128], True, True)
                S.tt("dve", A, A[:, :], psc, psc[:, :], Mk, Mk[:, :], OP.mult)

            def O():
                for s in range(2):
                    S.ts("dve", Sfbf, Sfbf[:, :, s, :], Sst["f"], Sst["f"][:, :, :], hm[:, s:s + 1], None, OP.mult, extra=[hm])
                po = S.bank()
                for h in range(4):
                    p, s = h // 2, h % 2
                    oap = po[:, h * 128:(h + 1) * 128]
                    S.mm(po, oap, Af, Af[:, h * 128:(h + 1) * 128], vbf, vbf[:, h * 128:(h + 1) * 128], True, False)
                    S.mm(po, oap, Ab, Ab[:, h * 128:(h + 1) * 128], vbf, vbf[:, h * 128:(h + 1) * 128], False, False)
                    S.mm(po, oap, qtl, qtl[:, p * 128:(p + 1) * 128], Sfbf, Sfbf[:, p, s, :], False, False)
                    S.mm(po, oap, qtl, qtl[:, (2 + p) * 128:(3 + p) * 128], Sbbf, Sbbf[:, n, p, s, :], False, True)
                su_apply("f", khat, vbf, etot)
                for h in range(4):
                    S.act(junk, junk[:, :], po, po[:, h * 128:(h + 1) * 128], AF.Square, accum=ssq[:, h:h + 1], accb=ssq)
                S.act(ssq, ssq[:, :], ssq, ssq[:, :], AF.Sqrt, bias=C["eps"][:, 0:1], scale=1.0 / 128, extra=[C["eps"]])
                S.op("dve", lambda e: e.reciprocal(out=ssq[:, :], in_=ssq[:, :]), reads=[ssq], writes=[ssq])
                for h in range(4):
                    S.ts("dve", on, on[:, h * 128:(h + 1) * 128], po, po[:, h * 128:(h + 1) * 128], ssq[:, h:h + 1], None, OP.mult, extra=[ssq])
                S.tt("pool", on, on[:, :], on, on[:, :], normg4, normg4[:, :], OP.mult)
                S.tt("dve", on, on[:, :], on, on[:, :], sg, sg[:, :], OP.mult)
                pt = S.bank()
                for j in range(4):
                    S.tr(pt, pt[:, j * 128:(j + 1) * 128], on, on[:, j * 128:(j + 1) * 128], C["ident"], C["ident"][:, :])
                y = ymt[n % 2]
                S.cp("act", y, y[:, :], pt, pt[:, :])
                S.dma("act", ymT_d[n][:, 0:4, :], y[:, :].rearrange("p (a b) -> p a b", a=4), reads=[y], writes=[ymT_d])

            return O

        pend = None
        for n in range(DEBUG.get("nfwd", 32)):
            o_ = fwd_chunk(n)
            if pend is not None:
                pend()
            pend = o_
        pend()
        S.barrier()


HY_L = 8192
HY_N = 16384
HY_DELTAS = np.abs(np.linspace(np.log(1e-2) / 1.5, np.log(1e-2) / 0.3, 512, dtype=np.float32)).astype(np.float64)


def hyena_tables():
    import ml_dtypes
    bf = ml_dtypes.bfloat16
    pi_idx = np.arange(128)
    tlo = 2 * (pi_idx % 64) + pi_idx // 64
    k = np.arange(128)
    T = {}
    th = np.arange(128)
    ph = 2 * np.pi * np.outer(th, k) / 128.0
    F1f = np.concatenate([np.cos(ph), -np.sin(ph)], 1)
    T["F1f"] = F1f.astype(bf)
    T["F1d"] = F1f[np.arange(128) % 64].astype(bf)
    tw = 2 * np.pi * np.outer(tlo, k) / HY_N
    c, s = np.cos(tw), np.sin(tw)
    T["TWc4"] = np.tile(np.concatenate([c, c], 1)[:, None, :], (1, 4, 1)).reshape(128, 1024).astype(bf)
    T["TWs4"] = np.tile(np.concatenate([s, s], 1)[:, None, :], (1, 4, 1)).reshape(128, 1024).astype(bf)
    c2, s2 = c.T, s.T
    T["TW2c4"] = np.tile(np.concatenate([c2, c2], 1)[:, None, :], (1, 4, 1)).reshape(128, 1024).astype(bf)
    T["TW2s4"] = np.tile(np.concatenate([s2, s2], 1)[:, None, :], (1, 4, 1)).reshape(128, 1024).astype(bf)
    p2 = 2 * np.pi * np.outer(tlo, k) / 128.0
    T["C2"] = np.cos(p2).astype(bf); T["S2"] = np.sin(p2).astype(bf); T["nS2"] = (-np.sin(p2)).astype(bf); T["nC2"] = (-np.cos(p2)).astype(bf)
    p3 = p2.T
    T["R3a"] = np.concatenate([np.cos(p3), np.sin(p3)], 1).astype(bf)
    T["R3b"] = np.concatenate([-np.sin(p3), np.cos(p3)], 1).astype(bf)
    T["nR3a"] = (-np.concatenate([np.cos(p3), np.sin(p3)], 1)).astype(bf)
    p4 = 2 * np.pi * np.outer(k, np.arange(64)) / 128.0
    T["C4"] = np.cos(p4).astype(bf); T["nS4"] = (-np.sin(p4)).astype(bf); T["nC4"] = (-np.cos(p4)).astype(bf)
    tau = 128 * th[:, None] + tlo[None, :]
    lag = np.where(tau < HY_L, tau, HY_N - tau)
    lag = np.where(tau == HY_L, 0, lag)
    T["tnl"] = (lag / (HY_L - 1.0)).astype(np.float32)
    lagq = lag.T.reshape(-1).astype(np.float64)
    bands = 16
    f = np.linspace(1e-4, bands - 1, bands)
    ang = (2.0 * np.pi * lagq / HY_L)[:, None] * f[None, :]
    z = np.concatenate([(lagq / (HY_L - 1.0))[:, None], np.cos(ang), -np.sin(ang)], -1)
    T["zT"] = np.ascontiguousarray(z.T).astype(np.float32)
    return T


def phase_A2(S, io, C, hT2_d, ymT_d):
    PI = float(np.pi)
    with ExitStack() as ls:
        tb = {}
        for nm, shp in (("F1f", [128, 256]), ("F1d", [128, 256]), ("TWc4", [128, 1024]), ("TWs4", [128, 1024]), ("TW2c4", [128, 1024]),
                        ("TW2s4", [128, 1024]), ("C2", [128, 128]), ("S2", [128, 128]), ("nS2", [128, 128]), ("R3a", [128, 256]),
                        ("R3b", [128, 256]), ("C4", [128, 64]), ("nS4", [128, 64]), ("nC2", [128, 128]), ("nR3a", [128, 256]), ("nC4", [128, 64])):
            tb[nm] = S.sb("t_" + nm, shp, BF16, ls)
            S.dma("sp", tb[nm][:, :], io[nm], writes=[tb[nm]])
        tnl = S.sb("tnl_sb", [128, 128], F32, ls)
        S.dma("sp", tnl[:, :], io["tnl"], writes=[tnl])
        hid2T = S.sb("hid2T", [64, HY_N], BF16, ls)
        with ExitStack() as l2:
            w1 = S.sb("fw1", [33, 64], F32, l2)
            w2 = S.sb("fw2", [64, 64], F32, l2)
            fb = S.sb("ffb", [64, 4], F32, l2)
            zt = [S.sb(f"fzt{i}", [33, 512], F32, l2) for i in range(2)]
            pre = [S.sb(f"fpre{i}", [64, 512], F32, l2) for i in range(2)]
            tmp = S.sb("ftmp", [64, 512], F32, l2)
            h1 = S.sb("fh1", [64, 512], F32, l2)
            S.dma("sp", w1[:, :], io["hy_w1"], writes=[w1])
            S.dma("sp", w2[:, :], io["hy_w2"], writes=[w2])
            S.dma("sp", fb[:, 0:3], io["hy_fb"], writes=[fb])
            S.tt("dve", fb, fb[:, 0:1], fb, fb[:, 0:1], fb, fb[:, 2:3], OP.mult)
            S.tt("dve", fb, fb[:, 1:2], fb, fb[:, 1:2], fb, fb[:, 2:3], OP.mult)

            def sin_layer(ps, bcol, out_b, out_ap, k):
                p = pre[k % 2]
                S.ts("dve", p, p[:, :], ps, ps[0:64, :], fb[:, 2:3], fb[:, bcol:bcol + 1], OP.mult, OP.add, extra=[fb])
                S.ts("dve", tmp, tmp[:, :], p, p[:, :], PI, -2 * PI, OP.is_gt, OP.mult)
                S.tt("dve", p, p[:, :], p, p[:, :], tmp, tmp[:, :], OP.add)
                S.ts("dve", tmp, tmp[:, :], p, p[:, :], -PI, 2 * PI, OP.is_lt, OP.mult)
                S.tt("dve", p, p[:, :], p, p[:, :], tmp, tmp[:, :], OP.add)
                S.act(out_b, out_ap, p, p[:, :], AF.Sin)

            for blk in range(HY_N // 512):
                z = zt[blk % 2]
                S.dma("sp", z[:, :], io["zT"][:, blk * 512:(blk + 1) * 512], writes=[z])
                ps = S.bank()
                S.mm(ps, ps[0:64, :], w1, w1[:, :], z, z[:, :], True, True)
                sin_layer(ps, 0, h1, h1[:, :], blk)
                ps2 = S.bank()
                S.mm(ps2, ps2[0:64, :], w2, w2[:, :], h1, h1[:, :], True, True)
                sin_layer(ps2, 1, hid2T, hid2T[:, blk * 512:(blk + 1) * 512], blk + 1)
            S.barrier()
        UX = S.sb("UX", [128, 64, 256], BF16, ls)
        X2 = S.sb("X2", [128, 64, 128], BF16, ls)
        Z = S.sb("Zc", [128, 64, 128], BF16, ls)
        Y2 = S.sb("Y2", [128, 64, 128], BF16, ls)
        identb = S.sb("identb", [128, 128], BF16, ls)
        S.cp("dve", identb, identb[:, :], C["ident"], C["ident"][:, :])
        S.memset("dve", Y2, Y2[:, :, :], 0.0)
        for ps_i in range(DEBUG.get("a2_passes", 4)):
            c0 = ps_i * 128
            with ExitStack() as l2:
                Wr = S.sb("hWr", [128, 8, 384], BF16, l2)
                Wt = [S.sb(f"hWt{i}", [128, 8, 384], BF16, l2) for i in range(3)]
                cwb = [S.sb(f"hcw{i}", [128, 384], F32, l2) for i in range(3)]
                cbb = S.sb("hcb", [128, 384], F32, l2)
                rowb = S.sb("hrowb", [1, 512], F32, l2)
                ring = [S.sb(f"hring{i}", [128, 8, 128], BF16, l2) for i in range(4)]
                S.dma("pool", Wr[:, :, :], io["w_hy_p"][ps_i].rearrange("(kc k) n -> k kc n", k=128), writes=[Wr])
                for i in range(3):
                    bcast_row(S, C, cwb[i], io["cw_p"][ps_i, i:i + 1, :], rowb, 384)
                    for kc in range(8):
                        S.tt("pool", Wt[i], Wt[i][:, kc, :], Wr, Wr[:, kc, :], cwb[i], cwb[i][:, :], OP.mult)
                bcast_row(S, C, cbb, io["cb_p"][ps_i], rowb, 384)
                tiles = {}
                shr = [S.sb(f"hshr{i}", [128, 8, 128], BF16, l2) for i in range(3)]
                shs = {}

                def get_tile(j):
                    if j not in tiles:
                        b = ring[j % 4]
                        S.dma("sp", b[:, :, :], hT2_d[j], writes=[b], reads=[hT2_d])
                        tiles[j] = b
                    return tiles[j]

                def get_sh(j):
                    if j not in shs:
                        b = shr[j % 3]
                        a_, c_ = get_tile(j - 1), get_tile(j)
                        S.cp("pool", b, b[:, :, 0:64], a_, a_[:, :, 64:128])
                        S.cp("pool", b, b[:, :, 64:128], c_, c_[:, :, 0:64])
                        shs[j] = b
                    return shs[j]

                for j in range(64):
                    cur = get_tile(j)
                    shl = get_sh(j) if j not in (0, 32) else None
                    shrt = get_sh(j + 1) if j not in (31, 63) else None
                    pb = S.bank()
                    mms = []
                    for kc in range(8):
                        mms.append((pb[:, 0:384], cur, cur[:, kc, :], Wt[1], Wt[1][:, kc, :]))
                        if shl is not None:
                            mms.append((pb[:, 0:384], shl, shl[:, kc, :], Wt[0], Wt[0][:, kc, :]))
                        else:
                            mms.append((pb[64:128, 0:384], cur, cur[:, kc, 0:64], Wt[0], Wt[0][:, kc, :]))
                        if shrt is not None:
                            mms.append((pb[:, 0:384], shrt, shrt[:, kc, :], Wt[2], Wt[2][:, kc, :]))
                        else:
                            mms.append((pb[0:64, 0:384], cur, cur[:, kc, 64:128], Wt[2], Wt[2][:, kc, :]))
                    last_center = [m_ for m_ in mms if m_[1] is cur and m_[3] is Wt[1]][-1]
                    mms.remove(last_center)
                    mms.append(last_center)
                    for mi, (o, lb, lap, rb, rap) in enumerate(mms):
                        S.mm(pb, o, lb, lap, rb, rap, mi == 0, mi == len(mms) - 1)
                    S.tt("dve", UX, UX[:, j, :], pb, pb[:, 0:256], cbb, cbb[:, 0:256], OP.add)
                    S.tt("dve", X2, X2[:, j, :], pb, pb[:, 256:384], cbb, cbb[:, 256:384], OP.add)
                S.barrier()
            with ExitStack() as l2:
                wo = S.sb("hwo", [64, 3, 2, 128], BF16, l2)
                dbn = S.sb("hdbn", [128, 256], F32, l2)
                rowb = S.sb("hrowb2", [1, 256], F32, l2)
                TS = S.sb("hTS", [128, 2, 16, 128], BF16, l2)
                Wn = S.sb("hWn", [128, 16, 128], BF16, l2)
                Hs = [S.sb(f"hH{i}", [128, 2, 8, 256], BF16, l2) for i in range(2)]
                CB = [[S.sb(f"hcb{c}_{i}", [128, 1024], BF16, l2) for i in range(3)] for c in range(4)]
                S.dma("pool", wo[:, :, :, :], io["wo_p"][ps_i], writes=[wo])
                bcast_row(S, C, dbn, io["db_p"][ps_i], rowb, 256)
                S.ts("dve", dbn, dbn[:, :], dbn, dbn[:, :], 1.0 / HY_N, None, OP.mult)
                NG = DEBUG.get("a2_groups", 16)

                def v4(bf):
                    return bf[:, :].rearrange("p (c r k) -> p c r k", c=4, r=2)

                def x4(bf):
                    return bf[:, :].rearrange("p (r c k) -> p r c k", r=2, c=4)

                def fwd_stages(st_, lhs_list, F1, K, bufs, pool_share=False):
                    b0, b1, b2 = bufs

                    def s0():
                        st_["pa"] = [S.bank(), S.bank()]
                        for ch in range(4):
                            p = st_["pa"][ch // 2]
                            for (lb, lap, psl) in lhs_list[ch]:
                                S.mm(p, p[psl, (ch % 2) * 256:(ch % 2 + 1) * 256], lb, lap, F1, F1[psl, :] if K == 64 else F1[:, :], True, True)

                    def s1():
                        if pool_share:
                            for h in range(2):
                                S.cp("act", b0, b0[:, h * 512:(h + 1) * 512], st_["pa"][h], st_["pa"][h][:, :])

                    def s2():
                        for h in range(2):
                            pa_ = st_["pa"][h]
                            cs = slice(h * 512, (h + 1) * 512)
                            S.tt("dve", b1, b1[:, cs], pa_, pa_[:, :], tb["TWc4"], tb["TWc4"][:, cs], OP.mult)
                            if not pool_share:
                                S.tt("dve", b2, b2[:, cs], pa_, pa_[:, :], tb["TWs4"], tb["TWs4"][:, cs], OP.mult)
                        if pool_share:
                            S.tt("pool", b2, b2[:, :], b0, b0[:, :], tb["TWs4"], tb["TWs4"][:, :], OP.mult)

                    def s3():
                        pr, pi_ = S.bank(), S.bank()
                        p1, p2 = v4(b1), v4(b2)
                        terms_r = ((tb["C2"], b1, p1[:, :, 0, :]), (tb["C2"], b2, p2[:, :, 1, :]), (tb["S2"], b1, p1[:, :, 1, :]), (tb["nS2"], b2, p2[:, :, 0, :]))
                        terms_i = ((tb["C2"], b1, p1[:, :, 1, :]), (tb["nC2"], b2, p2[:, :, 0, :]), (tb["nS2"], b1, p1[:, :, 0, :]), (tb["nS2"], b2, p2[:, :, 1, :]))
                        for po_, terms in ((pr, terms_r), (pi_, terms_i)):
                            for ti, (tbl, bb, ap_) in enumerate(terms):
                                S.mm(po_, po_[:, :], tbl, tbl[:, :], bb, ap_, ti == 0, ti == 3)
                        st_["pr"], st_["pi"] = pr, pi_

                    return [s0, s1, s2, s3]

                def filt_chain(g, o_, half, bufs):
                    H = Hs[g % 2]
                    st_ = {}
                    lhs = [[(TS, TS[:, o_, (g % 2) * 8 + half * 4 + ch, :], slice(0, 128))] for ch in range(4)]
                    stages = fwd_stages(st_, lhs, tb["F1f"], 128, bufs)

                    def s5():
                        pr, pi_ = st_["pr"], st_["pi"]
                        for ch in range(4):
                            cidx = o_ * 128 + g * 8 + half * 4 + ch
                            S.act(H, H[:, o_, half * 4 + ch, 0:128], pr, pr[:, ch * 128:(ch + 1) * 128], AF.Identity,
                                  bias=dbn[:, cidx:cidx + 1], scale=1.0 / HY_N, extra=[dbn])
                        S.act(H, H[:, o_, half * 4:(half + 1) * 4, 128:256], pi_, pi_[:, :].rearrange("p (c k) -> p c k", c=4), AF.Identity, scale=1.0 / HY_N)

                    return stages + [s5]

                def conv_chain(g, order, half, src, src_c0, bufs):
                    H = Hs[g % 2]
                    b0, b1, b2 = bufs
                    st_ = {}
                    lhs = []
                    for ch in range(4):
                        cc = src_c0 + g * 8 + half * 4 + ch
                        lhs.append([(src, src[0:64, :, cc], slice(0, 64)), (src, src[64:128, :, cc], slice(64, 128))])
                    stages = fwd_stages(st_, lhs, tb["F1d"], 64, bufs, pool_share=False)
                    Hv = H[:, order, half * 4:(half + 1) * 4, :].rearrange("p c (r k) -> p r c k", r=2)

                    def s5():
                        S.cp("act", b0, x4(b0)[:, 0, :, :], st_["pr"], st_["pr"][:, :].rearrange("p (c k) -> p c k", c=4))
                        S.cp("act", b0, x4(b0)[:, 1, :, :], st_["pi"], st_["pi"][:, :].rearrange("p (c k) -> p c k", c=4))

                    def s6():
                        S.tt("dve", b1, x4(b1)[:, 0, :, :], st_["pr"], st_["pr"][:, :].rearrange("p (c k) -> p c k", c=4), H, Hv[:, 0, :, :], OP.mult)
                        S.tt("dve", b1, x4(b1)[:, 1, :, :], st_["pi"], st_["pi"][:, :].rearrange("p (c k) -> p c k", c=4), H, Hv[:, 0, :, :], OP.mult)
                        for r in range(2):
                            S.tt("pool", b2, x4(b2)[:, r, :, :], b0, x4(b0)[:, r, :, :], H, Hv[:, 1, :, :], OP.mult)

                    def s7():
                        st_["pb3"] = [S.bank(), S.bank()]
                        m1, m2 = x4(b1), x4(b2)
                        for ch in range(4):
                            p = st_["pb3"][ch // 2]
                            o = p[:, (ch % 2) * 256:(ch % 2 + 1) * 256]
                            S.mm(p, o, b1, m1[:, 0, ch, :], tb["R3a"], tb["R3a"][:, :], True, False)
                            S.mm(p, o, b2, m2[:, 1, ch, :], tb["nR3a"], tb["nR3a"][:, :], False, False)
                            S.mm(p, o, b2, m2[:, 0, ch, :], tb["R3b"], tb["R3b"][:, :], False, False)
                            S.mm(p, o, b1, m1[:, 1, ch, :], tb["R3b"], tb["R3b"][:, :], False, True)

                    def s8():
                        for h in range(2):
                            S.cp("act", b0, b0[:, h * 512:(h + 1) * 512], st_["pb3"][h], st_["pb3"][h][:, :])

                    def s9():
                        for h in range(2):
                            pb_ = st_["pb3"][h]
                            cs = slice(h * 512, (h + 1) * 512)
                            S.tt("dve", b1, b1[:, cs], pb_, pb_[:, :], tb["TW2c4"], tb["TW2c4"][:, cs], OP.mult)
                        S.tt("pool", b2, b2[:, :], b0, b0[:, :], tb["TW2s4"], tb["TW2s4"][:, :], OP.mult)

                    return stages + [s5, s6, s7, s8, s9]

                def conv_tail(g, cbs, gate, gate_c0, dst, mrows):
                    p4 = S.bank()
                    for half in range(2):
                        b1, b2 = cbs[half][1], cbs[half][2]
                        q1, q2 = v4(b1), v4(b2)
                        for par in range(2):
                            o = p4[par * 64:par * 64 + mrows, half * 256:(half + 1) * 256]
                            cs = slice(par * 64, (par + 1) * 64)
                            S.mm(p4, o, tb["C4"], tb["C4"][:, 0:mrows], b1, q1[:, :, 0, cs], True, False)
                            S.mm(p4, o, tb["nC4"], tb["nC4"][:, 0:mrows], b2, q2[:, :, 1, cs], False, False)
                            S.mm(p4, o, tb["nS4"], tb["nS4"][:, 0:mrows], b2, q2[:, :, 0, cs], False, False)
                            S.mm(p4, o, tb["nS4"], tb["nS4"][:, 0:mrows], b1, q1[:, :, 1, cs], False, True)
                    for par in range(2):
                        rows = slice(par * 64, par * 64 + mrows)
                        gv = gate[rows, :, gate_c0 + g * 8:gate_c0 + g * 8 + 8].rearrange("p j c -> p c j")
                        dv = dst[rows, :, g * 8:g * 8 + 8].rearrange("p j c -> p c j")
                        S.tt("dve", dst, dv, p4, p4[rows, :].rearrange("p (c j) -> p c j", c=8), gate, gv, OP.mult)

                def ts_gen(g2):
                    for ch in range(16):
                        S.act(Wn, Wn[:, ch, :], tnl, tnl[:, :], AF.Exp, scale=-float(HY_DELTAS[c0 + g2 * 16 + ch]))
                    for blk in range(8):
                        p = S.bank()
                        for i_ in range(16):
                            pi_i = blk * 16 + i_
                            col = i_ * 32
                            S.mm(p, p[0:64, col:col + 32], hid2T, hid2T[:, pi_i * 128:pi_i * 128 + 64], wo, wo[:, 0, :, g2 * 16:(g2 + 1) * 16], True, True)
                            S.mm(p, p[64:128, col:col + 32], hid2T, hid2T[:, pi_i * 128 + 64:pi_i * 128 + 128], wo, wo[:, 1, :, g2 * 16:(g2 + 1) * 16], True, True)
                        for o_ in range(2):
                            src_v = p[:, :].rearrange("p (i o c) -> p o c i", i=16, o=2)[:, o_, :, :]
                            S.tt("dve", TS, TS[:, o_, :, blk * 16:(blk + 1) * 16], p, src_v, Wn, Wn[:, :, blk * 16:(blk + 1) * 16], OP.mult)
                    S.memset("dve", TS, TS[64:65, :, :, 0:1], 0.0)
                    pl0 = S.bank()
                    S.mm(pl0, pl0[0:1, 0:32], hid2T, hid2T[:, 0:1], wo, wo[:, 2, :, g2 * 16:(g2 + 1) * 16], True, True)
                    S.cp("dve", TS, TS[0:1, :, :, 0:1], pl0, pl0[0:1, 0:32].rearrange("p (a b c) -> p a b c", a=2, c=1))

                def interleave(chains):
                    for s_ in range(max(len(c_) for c_ in chains)):
                        for c_ in chains:
                            if s_ < len(c_):
                                c_[s_]()

                ts_gen(0)
                interleave([filt_chain(0, o_, half, CB[o_ * 2 + half]) for o_ in range(2) for half in range(2)])
                for g in range(NG):
                    nx = g + 1 < NG
                    if nx and (g + 1) % 2 == 0:
                        ts_gen((g + 1) // 2)
                    for order in range(2):
                        if order == 0:
                            chains = [conv_chain(g, 0, half, UX, 0, CB[half]) for half in range(2)]
                        else:
                            chains = [conv_chain(g, 1, half, Z, 0, CB[half]) for half in range(2)]
                        if nx:
                            chains += [filt_chain(g + 1, order, half, CB[2 + half]) for half in range(2)]
                        interleave(chains)
                        if order == 0:
                            conv_tail(g, CB, UX, 128, Z, 64)
                        else:
                            conv_tail(g, CB, X2, 0, Y2, 32)
                S.barrier()
            with ExitStack() as l2:
                Yb = S.sb("hYb", [128, 32, 128], BF16, l2)
                for j in range(64):
                    pt = S.bank()
                    ptb = pt[:, 0:64].bitcast(BF16)
                    S.tr(pt, ptb, Y2, Y2[:, j, :], identb, identb[:, :])
                    S.cp("act" if j % 2 else "dve", Yb, Yb[:, :, 2 * j:2 * j + 2].rearrange("c t p -> c p t"),
                         pt, ptb.rearrange("c (p t) -> c p t", p=2)[:, :, 0:32])
                for n in range(32):
                    S.dma("sp", ymT_d[n][:, 4 + ps_i, :], Yb[:, n, :], reads=[Yb], writes=[ymT_d])
                S.barrier()


def build(mode="full", NT=32):
    nc = bass.Bass("TRN2", target_bir_lowering=False)
    st = ExitStack()
    with st:
        S = Sched(nc, st)
        S.banks = [S.ps(f"bank{i}", [128, 512], F32) for i in range(8)]
        io = {}

        def inp(name, shape, dt=F32):
            io[name] = nc.dram_tensor(name, list(shape), dt, kind="ExternalInput").ap()

        inp("x_nat", [SEQ, D])
        io["x_own"] = io["x_nat"]
        inp("c2", [128, 8, 2])
        inp("ada_w", [D, 6 * D])
        inp("ada_b", [1, 6 * D])
        inp("ada_bT", [128, 6, 8])
        inp("ident", [128, 128])
        if mode in ("full", "B"):
            inp("w_out", [D, D])
            inp("ln1_g", [1, D]); inp("ln1_b", [1, D]); inp("ln2_g", [1, D]); inp("ln2_b", [1, D])
            inp("router_w", [D, NE]); inp("router_b", [1, NE])
            inp("exp_w1", [NE, D, 2 * D]); inp("exp_w2", [NE, D, D])
            inp("b1T", [128, NE, 8, 2]); inp("exp_b2", [NE, D])
            io["out"] = nc.dram_tensor("out", [OWN, D], F32, kind="ExternalOutput").ap()
        if mode in ("full", "A1", "A2", "A12"):
            inp("ctx_nat", [256, D])
            inp("w_q", [D, 256]); inp("w_k", [D, 256]); inp("w_v", [D, 512]); inp("w_g", [D, 512])
            for d in "fb":
                inp(f"aT_{d}", [16, D]); inp(f"wa_{d}", [16, 256])
            inp("ba", [1, 512]); inp("normg", [1, 128])
            for nm in ("Uincl", "Lincl", "Ustr", "Lstr"):
                inp(nm, [128, 128])
        if mode in ("full", "A2", "A12"):
            inp("x_perm", [SEQ, D])
            for nm, shp in (("F1f", [128, 256]), ("F1d", [128, 256]), ("TWc4", [128, 1024]), ("TWs4", [128, 1024]), ("TW2c4", [128, 1024]),
                            ("TW2s4", [128, 1024]), ("C2", [128, 128]), ("S2", [128, 128]), ("nS2", [128, 128]), ("R3a", [128, 256]),
                            ("R3b", [128, 256]), ("C4", [128, 64]), ("nS4", [128, 64]), ("nC2", [128, 128]), ("nR3a", [128, 256]), ("nC4", [128, 64])):
                inp(nm, shp, BF16)
            inp("tnl", [128, 128]); inp("zT", [33, HY_N])
            inp("hy_w1", [33, 64]); inp("hy_w2", [64, 64]); inp("hy_fb", [64, 3])
            inp("w_hy_p", [4, D, 384]); inp("cw_p", [4, 3, 384]); inp("cb_p", [4, 1, 384])
            inp("wo_p", [4, 64, 3, 2, 128]); inp("db_p", [4, 1, 256])
        C = {}
        C["ident"] = S.sb("ident_sb", [128, 128], F32)
        C["ones"] = S.sb("ones", [128, 128], F32)
        C["eps"] = S.sb("epsb", [128, 1], F32)
        S.dma("sp", C["ident"][:, :], io["ident"], writes=[C["ident"]])
        S.memset("dve", C["ones"], C["ones"][:, :], 1.0)
        S.memset("dve", C["eps"], C["eps"][:, :], EPS)
        gates = S.sb("gates", [128, 32, NE], F32)
        M = build_mod(S, st, io, C)
        if DEBUG.get("stop") == "mod":
            S.emit()
            return nc
        if mode == "B":
            inp("ymT_in", [32, 128, 8, 128], BF16)
            io["ymT"] = io["ymT_in"]
            io["ymT_buf"] = Buf("ymT_in_d")
        else:
            if mode == "full":
                ymT_d = S.dram("ymT_d", [32, 128, 8, 128], BF16)
            else:
                ymT_d = Buf("ymT_o_d", nc.dram_tensor("ymT_o", [32, 128, 8, 128], BF16, kind="ExternalOutput").ap())
            io["ymT"] = ymT_d.t
            io["ymT_buf"] = ymT_d
            hT_d = S.dram("hT_d", [66, 128, 8, 128], BF16)
            hT2_d = S.dram("hT2_d", [64, 128, 8, 128], BF16) if mode in ("full", "A2", "A12") else None
            a0 = PhaseA0(S, io, C, M, hT_d, hT2_d)
            a0.first()
            a0.finish()
            if DEBUG.get("stop") != "a0" and mode in ("full", "A1", "A12"):
                phase_A1(S, io, C, hT_d, ymT_d)
            if mode in ("full", "A2", "A12"):
                phase_A2(S, io, C, hT2_d, ymT_d)
        if mode in ("full", "B"):
            phase_B(S, io, C, M, gates, NT=NT)
        S.emit()
    return nc


def _core_inputs(inp, b, th, T):
    f32 = np.float32
    x = inp["x"][b]
    ctx = inp["ctx"][b]
    if th == 1:
        x = x[::-1]
        ctx = ctx[::-1]
    x = np.ascontiguousarray(x, dtype=f32)
    m = {}
    m["x_nat"] = x
    m["x_perm"] = np.ascontiguousarray(x.reshape(64, 128, D).transpose(1, 0, 2).reshape(SEQ, D))
    m["ctx_nat"] = np.ascontiguousarray(ctx, dtype=f32)
    c2 = np.zeros((128, 8, 2), f32)
    c2[:, :, 0] = inp["c"][b].reshape(8, 128).T
    c2[:, :, 1] = inp["c_ctx"].reshape(8, 128).T
    m["c2"] = c2
    m["ada_w"] = inp["ada_w"][0]
    m["ada_b"] = inp["ada_b"][0][None, :]
    m["ada_bT"] = np.ascontiguousarray(inp["ada_b"][0].reshape(6, 8, 128).transpose(2, 0, 1))
    m["ident"] = np.eye(128, dtype=f32)
    m["w_out"] = inp["w_out"][0]
    for k in ("ln1_g", "ln1_b", "ln2_g", "ln2_b", "router_b"):
        m[k] = inp[k].reshape(1, -1)
    m["router_w"] = inp["router_w"][0]
    m["exp_w1"] = inp["exp_w1"][0]
    m["exp_w2"] = inp["exp_w2"][0]
    m["b1T"] = np.ascontiguousarray(inp["exp_b1"][0].reshape(NE, 8, 128, 2).transpose(2, 0, 1, 3))
    m["exp_b2"] = inp["exp_b2"][0]
    w_in = inp["w_in"][0]
    m["w_q"] = np.ascontiguousarray(w_in[:, 0:256])
    m["w_k"] = np.ascontiguousarray(w_in[:, 256:512])
    m["w_v"] = np.ascontiguousarray(w_in[:, 512:1024])
    m["w_g"] = np.ascontiguousarray(w_in[:, 1024:1536])
    af, ab = w_in[:, 1536:1552], w_in[:, 1552:1568]
    waf, wab = inp["gla_wa_f"][0], inp["gla_wa_b"][0]
    baf, bab = inp["gla_ba_f"][0], inp["gla_ba_b"][0]
    if th == 1:
        af, ab, waf, wab, baf, bab = ab, af, wab, waf, bab, baf
    m["aT_f"] = np.ascontiguousarray(af.T)
    m["aT_b"] = np.ascontiguousarray(ab.T)
    m["wa_f"] = np.ascontiguousarray(waf)
    m["wa_b"] = np.ascontiguousarray(wab)
    m["ba"] = np.concatenate([baf, bab])[None, :].astype(f32)
    m["normg"] = inp["gla_norm_g"].reshape(1, 128)
    i = np.arange(128)
    m["Uincl"] = (i[:, None] <= i[None, :]).astype(f32)
    m["Lincl"] = (i[:, None] >= i[None, :]).astype(f32)
    m["Ustr"] = (i[:, None] < i[None, :]).astype(f32)
    m["Lstr"] = (i[:, None] > i[None, :]).astype(f32)
    m.update(T)
    cw = inp["hy_conv_w"][0]
    if th == 1:
        cw = cw[::-1]

    def per_pass(a):
        a3 = a.reshape(a.shape[:-1] + (3, 4, 128))
        a3 = np.moveaxis(a3, -2, 0)
        return np.ascontiguousarray(a3.reshape((4,) + a.shape[:-1] + (384,)))

    m["w_hy_p"] = per_pass(w_in[:, 1568:3104])
    m["cw_p"] = per_pass(cw)
    m["cb_p"] = per_pass(inp["hy_conv_b"][0][None, :])
    wout = inp["hy_flt_wout"][0].reshape(64, 2, 2, 512)
    dA, dB = (0, 1) if th == 0 else (1, 0)
    slots = np.stack([wout[:, :, dA, :], wout[:, :, dB, :], wout[:, :, 0, :]], 1)
    m["wo_p"] = np.ascontiguousarray(slots.reshape(64, 3, 2, 4, 128).transpose(3, 0, 1, 2, 4))
    m["db_p"] = np.ascontiguousarray(inp["hy_bias_d"][0].reshape(2, 4, 128).transpose(1, 0, 2).reshape(4, 1, 256))
    m["hy_w1"] = inp["hy_flt_w1"][0]
    m["hy_w2"] = inp["hy_flt_w2"][0]
    m["hy_fb"] = np.ascontiguousarray(np.stack([inp["hy_flt_b1"][0], inp["hy_flt_b2"][0], inp["hy_flt_freq"][0]], 1))
    return m


def kernel(**inputs):
    inp = {k: np.asarray(v) for k, v in inputs.items()}
    T = hyena_tables()
    nc = build("full")
    in_maps = [_core_inputs(inp, core // 2, core % 2, T) for core in range(8)]
    res = run_bass_kernel_spmd(nc, in_maps, core_ids=list(range(8)))
    out = np.empty((4, SEQ, D), np.float32)
    for core in range(8):
        b, th = core // 2, core % 2
        o = np.asarray(res.results[core]["out"])
        if th == 0:
            out[b, :OWN] = o
        else:
            out[b, OWN:] = o[::-1]
    return out
```
